# Optimizing a Trainium2 kernel written in Bass

```python
import math
import jax, jax.numpy as jnp
from jax import lax
import numpy as np

D_MODEL = 2048
BATCH = 8
SEQ = 4096
DEPTH = 4

GRID_W = 64
CTX_LEN = 256
MIX_W = D_MODEL
GROUP_W = MIX_W // 4
HEAD_DIM = 128
ATT_Q_HEADS = GROUP_W // HEAD_DIM
ATT_KV_HEADS = ATT_Q_HEADS // 2
ATT_REP = ATT_Q_HEADS // ATT_KV_HEADS
Q_BLOCK = 128
ROPE_THETA = 10000.0
ROPE_PAIRS_PER_AXIS = HEAD_DIM // 4
CONV_W = 4
CONV_PAD = (2, 1)
LRU_W = GROUP_W
LRU_BLOCKS = 4
LRU_BLOCK_W = LRU_W // LRU_BLOCKS
LRU_C = 8.0
FN_W = GROUP_W
FN_GROUPS = 4
FN_GROUP_W = FN_W // FN_GROUPS
SSD_W = GROUP_W
SSD_HEAD_DIM = 64
SSD_HEADS = SSD_W // SSD_HEAD_DIM
SSD_GROUPS = 2
SSD_HPG = SSD_HEADS // SSD_GROUPS
SSD_STATE = 128
SSD_CHUNK = 128
SSD_CONV_CH = SSD_W + 2 * SSD_GROUPS * SSD_STATE
N_EXPERTS = 32
TOP_K = 4
EXPERT_FF = 768
SWIGLU_LIMIT = 7.0
SWIGLU_ALPHA = 1.702
MOE_BLOCK = 128
EPS = 1e-6
IN_SPLIT_SIZES = (ATT_Q_HEADS * HEAD_DIM, ATT_KV_HEADS * HEAD_DIM, ATT_KV_HEADS * HEAD_DIM,
                  LRU_W, LRU_W, FN_W, SSD_W, SSD_CONV_CH, 2 * SSD_HEADS)
D_IN = sum(IN_SPLIT_SIZES)
F32 = jnp.float32

kernel_name = 'hymba_style_hybrid_dit_moe'


def _rmsnorm(x, g):
    xf = x.astype(F32)
    y = xf * lax.rsqrt(jnp.mean(xf * xf, axis=-1, keepdims=True) + EPS)
    return (y * g.astype(F32)).astype(x.dtype)


def _modulate(h, shift, scale):
    return h * (1 + scale) + shift


def _rev(t, d):
    return t[:, ::-1] if d == 1 else t


def _axial_rope_tables(n_tokens):
    rows = n_tokens // GRID_W
    row = jnp.repeat(jnp.arange(rows), GRID_W).astype(F32)
    col = jnp.tile(jnp.arange(GRID_W), rows).astype(F32)
    inv = ROPE_THETA ** (-jnp.arange(ROPE_PAIRS_PER_AXIS, dtype=F32) / ROPE_PAIRS_PER_AXIS)
    ang = jnp.concatenate([row[:, None] * inv, col[:, None] * inv], axis=-1)
    ang = jnp.concatenate([ang, ang], axis=-1)
    return jnp.cos(ang), jnp.sin(ang)


def _apply_rope(x, cos, sin):
    x1, x2 = jnp.split(x, 2, axis=-1)
    rot = jnp.concatenate([-x2, x1], axis=-1)
    return (x.astype(F32) * cos[:, None] + rot.astype(F32) * sin[:, None]).astype(x.dtype)


def _dwconv(u, w, b):
    y = lax.conv_general_dilated(u, w[:, None, :].astype(u.dtype), window_strides=(1,),
                                 padding=(CONV_PAD,), dimension_numbers=('NWC', 'WIO', 'NWC'),
                                 feature_group_count=u.shape[-1])
    return y + b.astype(u.dtype)


def _attention(q, k, v, q_c, k_c, v_c, q_norm, k_norm, cos, sin):
    B, L, _ = q.shape

    def heads(t, n):
        return t.reshape(t.shape[0], t.shape[1], n, HEAD_DIM)

    q = _apply_rope(_rmsnorm(heads(q, ATT_Q_HEADS), q_norm), cos, sin)
    k = _apply_rope(_rmsnorm(heads(k, ATT_KV_HEADS), k_norm), cos, sin)
    v = heads(v, ATT_KV_HEADS)
    q_c = _rmsnorm(heads(q_c, ATT_Q_HEADS), q_norm)
    k_c = _rmsnorm(heads(k_c, ATT_KV_HEADS), k_norm)
    v_c = heads(v_c, ATT_KV_HEADS)
    scale = HEAD_DIM ** -0.5

    def attend(qb, keys, vals):
        qb = qb.reshape(qb.shape[0], qb.shape[1], ATT_KV_HEADS, ATT_REP, HEAD_DIM)
        s = jnp.einsum('bqkrd,bskd->bkrqs', qb, keys).astype(F32) * scale
        p = jax.nn.softmax(s, axis=-1).astype(vals.dtype)
        o = jnp.einsum('bkrqs,bskd->bqkrd', p, vals)
        return o.reshape(o.shape[0], o.shape[1], ATT_Q_HEADS * HEAD_DIM)

    y_c = attend(q_c, k_c, v_c)
    k_all = jnp.concatenate([k_c, k], axis=1)
    v_all = jnp.concatenate([v_c, v], axis=1)
    n_blk = L // Q_BLOCK
    q_blocks = jnp.moveaxis(q.reshape(B, n_blk, Q_BLOCK, ATT_Q_HEADS, HEAD_DIM), 1, 0)
    y = lax.map(lambda qb: attend(qb, k_all, v_all), q_blocks)
    y = jnp.moveaxis(y, 0, 1).reshape(B, L, ATT_Q_HEADS * HEAD_DIM)
    return y_c, y


def _lru_coeffs(u, wa, ba, wx, bx, lam):
    ub = u.reshape(u.shape[0], u.shape[1], LRU_BLOCKS, LRU_BLOCK_W)
    r = jax.nn.sigmoid(jnp.einsum('blnc,ncd->blnd', ub, wa.astype(F32)).reshape(u.shape) + ba.astype(F32))
    i = jax.nn.sigmoid(jnp.einsum('blnc,ncd->blnd', ub, wx.astype(F32)).reshape(u.shape) + bx.astype(F32))
    log_a = -LRU_C * r * jax.nn.softplus(-lam.astype(F32))
    a = jnp.exp(log_a)
    b = jnp.sqrt(-jnp.expm1(2.0 * log_a)) * (i * u)
    return a, b


def _linear_scan(a, b, h0):
    def combine(left, right):
        a_l, b_l = left
        a_r, b_r = right
        return a_l * a_r, a_r * b_l + b_r

    a_cum, b_cum = lax.associative_scan(combine, (a, b), axis=1)
    return b_cum + a_cum * h0[:, None, :]


def _rglru_bidir(u_c, u, wa, ba, wx, bx, lam):
    u_c32, u32 = u_c.astype(F32), u.astype(F32)
    outs_c, outs = [], []
    for d in range(2):
        a_c, b_c = _lru_coeffs(_rev(u_c32, d), wa[d], ba[d], wx[d], bx[d], lam[d])
        h_c = _linear_scan(a_c, b_c, jnp.zeros_like(u_c32[:, 0]))
        a_l, b_l = _lru_coeffs(_rev(u32, d), wa[d], ba[d], wx[d], bx[d], lam[d])
        h = _linear_scan(a_l, b_l, h_c[:, -1])
        outs_c.append(_rev(h_c, d))
        outs.append(_rev(h, d))
    return (outs_c[0] + outs_c[1]).astype(u_c.dtype), (outs[0] + outs[1]).astype(u.dtype)


def _fourier(u):
    B, L, _ = u.shape
    uf = u.astype(F32).reshape(B, L, FN_GROUPS, FN_GROUP_W)
    y = jnp.fft.fft2(uf, axes=(1, 3), norm='ortho').real
    return y.reshape(B, L, FN_W).astype(u.dtype)


def _ssd_chunked(x, dt, A, Bm, Cm, h0):
    b, L, g, r, p = x.shape
    n = Bm.shape[-1]
    c = L // SSD_CHUNK
    xdt = (x * dt[..., None]).reshape(b, c, SSD_CHUNK, g, r, p)
    Bc = Bm.reshape(b, c, SSD_CHUNK, g, n)
    Cc = Cm.reshape(b, c, SSD_CHUNK, g, n)
    a = jnp.transpose((dt * A).reshape(b, c, SSD_CHUNK, g, r), (0, 3, 4, 1, 2))
    acs = jnp.cumsum(a, axis=-1)
    tril = jnp.tril(jnp.ones((SSD_CHUNK, SSD_CHUNK), dtype=bool))
    seg = acs[..., :, None] - acs[..., None, :]
    Lmat = jnp.exp(jnp.where(tril, seg, -jnp.inf))
    y_diag = jnp.einsum('bclgn,bcsgn,bgrcls,bcsgrp->bclgrp', Cc, Bc, Lmat, xdt)
    decay_states = jnp.exp(acs[..., -1:] - acs)
    states = jnp.einsum('bclgn,bgrcl,bclgrp->bcgrpn', Bc, decay_states, xdt)
    states = jnp.concatenate([h0[:, None], states], axis=1)
    tot = jnp.concatenate([jnp.zeros_like(acs[..., :1, -1]), acs[..., -1]], axis=-1)
    ccs = jnp.cumsum(tot, axis=-1)
    tril_c = jnp.tril(jnp.ones((c + 1, c + 1), dtype=bool))
    dchunk = jnp.exp(jnp.where(tril_c, ccs[..., :, None] - ccs[..., None, :], -jnp.inf))
    new_states = jnp.einsum('bgrzc,bcgrpn->bzgrpn', dchunk, states)
    states_in, final = new_states[:, :-1], new_states[:, -1]
    y_off = jnp.einsum('bclgn,bcgrpn,bgrcl->bclgrp', Cc, states_in, jnp.exp(acs))
    return (y_diag + y_off).reshape(b, L, g, r, p), final


def _ssd_split(xbc, dt):
    B, L = xbc.shape[:2]
    xs, bm, cm = jnp.split(xbc.astype(F32), [SSD_W, SSD_W + SSD_GROUPS * SSD_STATE], axis=-1)
    xs = xs.reshape(B, L, SSD_GROUPS, SSD_HPG, SSD_HEAD_DIM)
    bm = bm.reshape(B, L, SSD_GROUPS, SSD_STATE)
    cm = cm.reshape(B, L, SSD_GROUPS, SSD_STATE)
    dt = dt.astype(F32).reshape(B, L, 2, SSD_GROUPS, SSD_HPG)
    return xs, bm, cm, dt


def _ssd_bidir(xbc_c, dt_c, z_c, xbc, dt, z, a_log, dt_bias, d_skip, norm_g):
    xs_c, b_c, c_c, dr_c = _ssd_split(xbc_c, dt_c)
    xs, b_l, c_l, dr = _ssd_split(xbc, dt)
    dsk = d_skip.astype(F32).reshape(SSD_GROUPS, SSD_HPG, 1)
    ys_c, ys = [xs_c * dsk], [xs * dsk]
    for d in range(2):
        A = -jnp.exp(a_log[d].astype(F32)).reshape(SSD_GROUPS, SSD_HPG)
        bias = dt_bias[d].astype(F32).reshape(SSD_GROUPS, SSD_HPG)
        dtc = jax.nn.softplus(dr_c[:, :, d] + bias)
        dtl = jax.nn.softplus(dr[:, :, d] + bias)
        h0 = jnp.zeros((xs_c.shape[0], SSD_GROUPS, SSD_HPG, SSD_HEAD_DIM, SSD_STATE), F32)
        y_cd, h_ctx = _ssd_chunked(_rev(xs_c, d), _rev(dtc, d), A, _rev(b_c, d), _rev(c_c, d), h0)
        y_ld, _ = _ssd_chunked(_rev(xs, d), _rev(dtl, d), A, _rev(b_l, d), _rev(c_l, d), h_ctx)
        ys_c.append(_rev(y_cd, d))
        ys.append(_rev(y_ld, d))

    def gated_norm(parts, zz):
        y = (parts[0] + parts[1] + parts[2]).reshape(zz.shape)
        return _rmsnorm(y * jax.nn.silu(zz.astype(F32)), norm_g).astype(zz.dtype)

    return gated_norm(ys_c, z_c), gated_norm(ys, z)


def _token_mixers(u_c, u, cos, sin, q_norm, k_norm, lru_conv_w, lru_conv_b, lru_wa, lru_ba,
                  lru_wx, lru_bx, lru_lam, ssd_conv_w, ssd_conv_b, ssd_a_log, ssd_dt_bias,
                  ssd_d, ssd_norm):
    cuts = np.cumsum(IN_SPLIT_SIZES)[:-1].tolist()
    q_c, k_c, v_c, lx_c, lg_c, f_c, z_c, xbc_c, dt_c = jnp.split(u_c, cuts, axis=-1)
    q, k, v, lx, lg, f, z, xbc, dt = jnp.split(u, cuts, axis=-1)
    att_c, att = _attention(q, k, v, q_c, k_c, v_c, q_norm, k_norm, cos, sin)
    rec_c, rec = _rglru_bidir(_dwconv(lx_c, lru_conv_w, lru_conv_b), _dwconv(lx, lru_conv_w, lru_conv_b),
                              lru_wa, lru_ba, lru_wx, lru_bx, lru_lam)
    lru_c = rec_c * jax.nn.gelu(lg_c)
    lru = rec * jax.nn.gelu(lg)
    four_c, four = _fourier(f_c), _fourier(f)
    ssd_c, ssd = _ssd_bidir(jax.nn.silu(_dwconv(xbc_c, ssd_conv_w, ssd_conv_b)), dt_c, z_c,
                            jax.nn.silu(_dwconv(xbc, ssd_conv_w, ssd_conv_b)), dt, z,
                            ssd_a_log, ssd_dt_bias, ssd_d, ssd_norm)
    feat_c = jnp.concatenate([att_c, lru_c, four_c, ssd_c], axis=-1)
    feat = jnp.concatenate([att, lru, four, ssd], axis=-1)
    return feat_c, feat


def _moe(h, router_w, router_b, w_gu, b_gu, w_dn, b_dn):
    T = h.shape[0]
    TK = T * TOP_K
    logits = (h @ router_w + router_b).astype(F32)
    top_v, top_i = lax.top_k(logits, TOP_K)
    gates = jax.nn.softmax(top_v, axis=-1)
    flat_e = top_i.reshape(-1)
    flat_t = jnp.repeat(jnp.arange(T, dtype=jnp.int32), TOP_K)
    flat_g = gates.reshape(-1)
    order = jnp.argsort(flat_e)
    se, st, sg = flat_e[order], flat_t[order], flat_g[order]
    counts = jnp.bincount(flat_e, length=N_EXPERTS)
    starts = jnp.cumsum(counts) - counts
    pcounts = (counts + MOE_BLOCK - 1) // MOE_BLOCK * MOE_BLOCK
    pends = jnp.cumsum(pcounts)
    pstarts = pends - pcounts
    dest = pstarts[se] + (jnp.arange(TK) - starts[se])
    n_blocks = -(-TK // MOE_BLOCK) + N_EXPERTS
    P = n_blocks * MOE_BLOCK
    tok = jnp.zeros((P,), jnp.int32).at[dest].set(st)
    gw = jnp.zeros((P,), F32).at[dest].set(sg)
    blk_e = jnp.minimum(jnp.searchsorted(pends, jnp.arange(n_blocks) * MOE_BLOCK, side='right'),
                        N_EXPERTS - 1)

    def run(args):
        e, idx, g = args
        xb = h[idx]
        gu = xb @ w_gu[e] + b_gu[e]
        gate = jnp.minimum(gu[..., ::2], SWIGLU_LIMIT)
        up = jnp.clip(gu[..., 1::2], -SWIGLU_LIMIT, SWIGLU_LIMIT)
        act = gate * jax.nn.sigmoid(SWIGLU_ALPHA * gate) * (up + 1)
        out = act @ w_dn[e] + b_dn[e]
        return out * g.astype(h.dtype)[:, None]

    outs = lax.map(run, (blk_e, tok.reshape(n_blocks, MOE_BLOCK), gw.reshape(n_blocks, MOE_BLOCK)))
    return jnp.zeros_like(h).at[tok].add(outs.reshape(P, h.shape[1]).astype(h.dtype))


def _layer(x, xc, mod, mod_c, cos, sin, update_ctx, norm1, norm2, w_in, w_out, q_norm, k_norm,
           lru_conv_w, lru_conv_b, lru_wa, lru_ba, lru_wx, lru_bx, lru_lam, ssd_conv_w, ssd_conv_b,
           ssd_a_log, ssd_dt_bias, ssd_d, ssd_norm, router_w, router_b, exp_w_gu, exp_b_gu,
           exp_w_dn, exp_b_dn):
    sh1, sc1, g1, sh2, sc2, g2 = jnp.split(mod[:, None, :], 6, axis=-1)
    sh1c, sc1c, g1c, sh2c, sc2c, g2c = jnp.split(mod_c, 6, axis=-1)
    h = _modulate(_rmsnorm(x, norm1), sh1, sc1)
    hc = _modulate(_rmsnorm(xc, norm1), sh1c, sc1c)
    feat_c, feat = _token_mixers(hc @ w_in, h @ w_in, cos, sin, q_norm, k_norm, lru_conv_w, lru_conv_b,
                                 lru_wa, lru_ba, lru_wx, lru_bx, lru_lam, ssd_conv_w, ssd_conv_b,
                                 ssd_a_log, ssd_dt_bias, ssd_d, ssd_norm)
    x = x + g1 * (feat @ w_out)
    tokens = _modulate(_rmsnorm(x, norm2), sh2, sc2).reshape(-1, x.shape[-1])
    if update_ctx:
        xc = xc + g1c * (feat_c @ w_out)
        h2c = _modulate(_rmsnorm(xc, norm2), sh2c, sc2c).reshape(-1, xc.shape[-1])
        n_c = h2c.shape[0]
        ffn = _moe(jnp.concatenate([h2c, tokens], axis=0), router_w, router_b, exp_w_gu, exp_b_gu,
                   exp_w_dn, exp_b_dn)
        xc = xc + g2c * ffn[:n_c].reshape(xc.shape)
        ffn = ffn[n_c:]
    else:
        ffn = _moe(tokens, router_w, router_b, exp_w_gu, exp_b_gu, exp_w_dn, exp_b_dn)
    x = x + g2 * ffn.reshape(x.shape)
    return x, xc


def setup_inputs(seed: int = 0) -> dict:
    key = jax.random.key(seed)
    keys = iter(jax.random.split(key, 31))
    L = DEPTH

    def normal(shape, std):
        return std * jax.random.normal(next(keys), shape, F32)

    def gain(shape):
        return 1.0 + normal(shape, 0.02)

    def uniform(shape, lo, hi):
        return jax.random.uniform(next(keys), shape, F32, lo, hi)

    inp = {}
    inp['x'] = normal((BATCH, SEQ, D_MODEL), 1.0)
    inp['c'] = normal((BATCH, D_MODEL), 1.0)
    inp['ctx'] = normal((BATCH, CTX_LEN, D_MODEL), 1.0)
    inp['c_ctx'] = normal((D_MODEL,), 1.0)
    inp['w_mod'] = normal((L, D_MODEL, 6 * D_MODEL), 0.5 * D_MODEL ** -0.5)
    inp['b_mod'] = normal((L, 6 * D_MODEL), 0.02)
    inp['norm1'] = gain((L, D_MODEL))
    inp['norm2'] = gain((L, D_MODEL))
    inp['w_in'] = normal((L, D_MODEL, D_IN), D_MODEL ** -0.5)
    inp['w_out'] = normal((L, MIX_W, D_MODEL), MIX_W ** -0.5)
    inp['q_norm'] = gain((L, HEAD_DIM))
    inp['k_norm'] = gain((L, HEAD_DIM))
    inp['lru_conv_w'] = normal((L, CONV_W, LRU_W), CONV_W ** -0.5)
    inp['lru_conv_b'] = normal((L, LRU_W), 0.02)
    inp['lru_wa'] = normal((L, 2, LRU_BLOCKS, LRU_BLOCK_W, LRU_BLOCK_W), LRU_BLOCK_W ** -0.5)
    inp['lru_ba'] = normal((L, 2, LRU_W), 0.02)
    inp['lru_wx'] = normal((L, 2, LRU_BLOCKS, LRU_BLOCK_W, LRU_BLOCK_W), LRU_BLOCK_W ** -0.5)
    inp['lru_bx'] = normal((L, 2, LRU_W), 0.02)
    a0 = uniform((L, 2, LRU_W), 0.9, 0.999)
    inp['lru_lam'] = jnp.log(a0) - jnp.log1p(-a0)
    inp['ssd_conv_w'] = normal((L, CONV_W, SSD_CONV_CH), CONV_W ** -0.5)
    inp['ssd_conv_b'] = normal((L, SSD_CONV_CH), 0.02)
    inp['ssd_a_log'] = jnp.log(uniform((L, 2, SSD_HEADS), 1.0, 16.0))
    dt0 = jnp.exp(uniform((L, 2, SSD_HEADS), math.log(1e-3), math.log(1e-1)))
    inp['ssd_dt_bias'] = dt0 + jnp.log(-jnp.expm1(-dt0))
    inp['ssd_d'] = gain((L, SSD_HEADS))
    inp['ssd_norm'] = gain((L, SSD_W))
    inp['router_w'] = normal((L, D_MODEL, N_EXPERTS), D_MODEL ** -0.5)
    inp['router_b'] = normal((L, N_EXPERTS), 0.01)
    inp['exp_w_gu'] = normal((L, N_EXPERTS, D_MODEL, 2 * EXPERT_FF), D_MODEL ** -0.5)
    inp['exp_b_gu'] = normal((L, N_EXPERTS, 2 * EXPERT_FF), 0.02)
    inp['exp_w_dn'] = normal((L, N_EXPERTS, EXPERT_FF, D_MODEL), EXPERT_FF ** -0.5)
    inp['exp_b_dn'] = normal((L, N_EXPERTS, D_MODEL), 0.02)
    return inp


def reference(x, c, ctx, c_ctx, w_mod, b_mod, norm1, norm2, w_in, w_out, q_norm, k_norm,
              lru_conv_w, lru_conv_b, lru_wa, lru_ba, lru_wx, lru_bx, lru_lam, ssd_conv_w,
              ssd_conv_b, ssd_a_log, ssd_dt_bias, ssd_d, ssd_norm, router_w, router_b,
              exp_w_gu, exp_b_gu, exp_w_dn, exp_b_dn):
    cos, sin = _axial_rope_tables(x.shape[1])
    s_c = jax.nn.silu(c)
    s_cc = jax.nn.silu(c_ctx)
    xc = ctx
    for l in range(DEPTH):
        mod = s_c @ w_mod[l] + b_mod[l]
        mod_c = s_cc @ w_mod[l] + b_mod[l]
        x, xc = _layer(x, xc, mod, mod_c, cos, sin, l < DEPTH - 1, norm1[l], norm2[l], w_in[l],
                       w_out[l], q_norm[l], k_norm[l], lru_conv_w[l], lru_conv_b[l], lru_wa[l],
                       lru_ba[l], lru_wx[l], lru_bx[l], lru_lam[l], ssd_conv_w[l], ssd_conv_b[l],
                       ssd_a_log[l], ssd_dt_bias[l], ssd_d[l], ssd_norm[l], router_w[l], router_b[l],
                       exp_w_gu[l], exp_b_gu[l], exp_w_dn[l], exp_b_dn[l])
    return x
```

```python
import math
from contextlib import ExitStack

import numpy as np
import ml_dtypes
import concourse.bass as bass
import concourse.mybir as mybir
from concourse.bass_utils import run_bass_kernel_spmd

F32 = mybir.dt.float32
BF16 = mybir.dt.bfloat16
AF = mybir.ActivationFunctionType
ALU = mybir.AluOpType
AX = mybir.AxisListType

D = 2048
DEPTH = 4
T = 4352
NCTX = 256
SEQ = 4096
D_IN = 4112
NE = 32
EFF = 768
EPS = 1e-6
TILES = [(0, 256)] + [(256 + 512 * i, 512) for i in range(8)]
NCH = T // 128

C_BMOD, C_N1, C_N2, C_QN, C_KN = 0, 96, 112, 128, 129
C_LCW, C_LCB, C_LBA, C_LBX, C_LLAM = 130, 146, 150, 158, 166
C_SCW, C_SCB, C_SD, C_SN, C_ALOG, C_DTB = 174, 206, 214, 218, 222, 223
C_BG, C_BU, NCOLS = 224, 416, 608
K_ONES, K_RM, K_ID, K_SEL, NK = 0, 128, 256, 384, 384 + 16 * 128

ENGINES = ("tensor", "vector", "scalar", "gpsimd", "sync")


class _Op:
    __slots__ = ("eng", "fn", "reads", "writes", "dma", "waits", "sig", "mm")

    def __init__(self, eng, fn, reads, writes, dma, mm):
        self.eng, self.fn, self.reads, self.writes, self.dma, self.mm = eng, fn, reads, writes, dma, mm
        self.waits = []
        self.sig = None


class SemPool:
    def __init__(self, nc, stack, n_dma=88):
        self.eng_sem = {e: stack.enter_context(nc.semaphore("pe_" + e)) for e in ENGINES}
        self.eng_val = {e: 0 for e in ENGINES}
        self.dma_sems = [stack.enter_context(nc.semaphore("pd_%d" % i)) for i in range(n_dma)]
        self.dma_val = [0] * n_dma


class Phase:
    def __init__(self, nc, pool, name):
        self.nc, self.pool, self.name = nc, pool, name
        self.ops = []
        self.dma_key2slot = {}
        self.st = ExitStack()

    def __enter__(self):
        self.st.__enter__()
        return self

    def __exit__(self, *a):
        if a[0] is None:
            self.emit()
        return self.st.__exit__(*a)

    def sb(self, name, shape, dt=F32):
        return self.st.enter_context(self.nc.sbuf_tensor("%s_%s" % (self.name, name), list(shape), dt))

    def ps(self, name, shape, dt=F32):
        return self.st.enter_context(self.nc.psum_tensor("%s_%s" % (self.name, name), list(shape), dt))

    def op(self, eng, fn, reads=(), writes=(), mm=False):
        self.ops.append(_Op(eng, fn, tuple(reads), tuple(writes), None, mm))

    def dma(self, eng, fn, key, reads=(), writes=()):
        self.ops.append(_Op(eng, fn, tuple(reads), tuple(writes), key, False))

    def mm(self, out, lhsT, rhs, start, stop, r, w):
        self.op("tensor", lambda e: e.matmul(out, lhsT=lhsT, rhs=rhs, start=start, stop=stop), r, w, mm=True)

    def tr(self, out, in_, ident, r, w):
        self.op("tensor", lambda e: e.transpose(out, in_, ident), r, w, mm=True)

    def act(self, out, in_, func, r, w, bias=None, scale=None):
        kw = {}
        if bias is not None:
            kw["bias"] = bias
        if scale is not None:
            kw["scale"] = scale
        self.op("scalar", lambda e: e.activation(out=out, in_=in_, func=func, **kw), r, w)

    def tt(self, eng, out, in0, in1, op, r, w):
        self.op(eng, lambda e: e.tensor_tensor(out=out, in0=in0, in1=in1, op=op), r, w)

    def ts(self, eng, out, in0, s1, s2, op0, op1, r, w):
        if op1 is None:
            self.op(eng, lambda e: e.tensor_scalar(out=out, in0=in0, scalar1=s1, scalar2=None, op0=op0), r, w)
        else:
            self.op(eng, lambda e: e.tensor_scalar(out=out, in0=in0, scalar1=s1, scalar2=s2, op0=op0, op1=op1), r, w)

    def stt(self, out, in0, scalar, in1, op0, op1, r, w):
        self.op("vector", lambda e: e.scalar_tensor_tensor(out=out, in0=in0, scalar=scalar, in1=in1, op0=op0, op1=op1), r, w)

    def cp(self, eng, out, in_, r, w):
        if eng == "scalar":
            self.op(eng, lambda e: e.copy(out=out, in_=in_), r, w)
        else:
            self.op(eng, lambda e: e.tensor_copy(out=out, in_=in_), r, w)

    def recip(self, out, in_, r, w):
        self.op("vector", lambda e: e.reciprocal(out=out, in_=in_), r, w)

    def scan(self, out, d0, d1, init, r, w):
        self.op("vector", lambda e: e.tensor_tensor_scan(out=out, data0=d0, data1=d1, initial=init, op0=ALU.mult, op1=ALU.add), r, w)

    def memset(self, eng, ap, val, w):
        self.op(eng, lambda e: e.memset(ap, val), (), w)

    def ld(self, out, in_, key, w, r=(), eng="sync"):
        self.dma(eng, lambda e: e.dma_start(out=out, in_=in_), key, reads=r, writes=w)

    def store(self, out, in_, key, r, w=(), eng="sync"):
        self.dma(eng, lambda e: e.dma_start(out=out, in_=in_), key, reads=r, writes=w)

    def emit(self):
        pool, ops = self.pool, self.ops
        last_w, readers, deps_of, needed = {}, {}, [], set()
        for i, o in enumerate(ops):
            deps = set()
            for r in o.reads:
                w = last_w.get(r)
                if w is not None:
                    deps.add(w)
            for w_ in o.writes:
                w = last_w.get(w_)
                if w is not None:
                    deps.add(w)
                deps.update(readers.get(w_, ()))
            deps.discard(i)
            fd = []
            for d in deps:
                od = ops[d]
                if od.mm and o.mm:
                    continue
                if od.dma is not None and o.dma is not None and od.dma == o.dma and od.eng == o.eng \
                        and not (set(od.writes) & set(o.reads)) and not (set(od.reads) & set(o.writes)):
                    continue
                fd.append(d)
                needed.add(d)
            deps_of.append(fd)
            for r in o.reads:
                readers.setdefault(r, []).append(i)
            for w_ in o.writes:
                last_w[w_] = i
                readers[w_] = []
        for i, o in enumerate(ops):
            if o.dma is not None:
                slot = self.dma_key2slot.get(o.dma)
                if slot is None:
                    slot = len(self.dma_key2slot)
                    assert slot < len(pool.dma_sems), "out of dma semaphores in " + self.name
                    self.dma_key2slot[o.dma] = slot
                pool.dma_val[slot] += 16
                o.sig = (pool.dma_sems[slot], 16, pool.dma_val[slot], ("d", slot))
            elif i in needed:
                pool.eng_val[o.eng] += 1
                o.sig = (pool.eng_sem[o.eng], 1, pool.eng_val[o.eng], ("e", o.eng))
        waited = {e: {} for e in ENGINES}
        cur = {}
        for i, o in enumerate(ops):
            wl = {}
            for d in deps_of[i]:
                s = ops[d].sig
                if s is None:
                    continue
                sem, _, val, sid = s
                if sid[0] == "d":
                    val = max(val, cur.get(sid, 0))
                if waited[o.eng].get(sid, 0) >= val:
                    continue
                if wl.get(sid, (None, 0))[1] < val:
                    wl[sid] = (sem, val)
            for sid, (sem, val) in wl.items():
                waited[o.eng][sid] = val
                o.waits.append((sem, val))
            if o.dma is not None:
                cur[o.sig[3]] = o.sig[2]
        tail = {e: [] for e in ENGINES}
        seen = set()
        for o in reversed(ops):
            if o.dma is not None:
                sem, _, val, sid = o.sig
                if sid in seen:
                    continue
                seen.add(sid)
                if waited[o.eng].get(sid, 0) < val:
                    tail[o.eng].append((sem, val))
        by_eng = {e: [o for o in ops if o.eng == e] for e in ENGINES}
        with self.nc.Block() as block:
            for e in ENGINES:
                lst, tl = by_eng[e], tail[e]
                if not lst and not tl:
                    continue

                def body(eng, lst=lst, tl=tl):
                    for o in lst:
                        for sem, val in o.waits:
                            eng.wait_ge(sem, val)
                        ins = o.fn(eng)
                        if o.sig is not None:
                            ins.then_inc(o.sig[0], o.sig[1])
                    for sem, val in tl:
                        eng.wait_ge(sem, val)

                getattr(block, e)(body)
        self.ops = []


class Ring:
    def __init__(self, items):
        self.items, self.i = items, 0

    def next(self):
        it = self.items[self.i % len(self.items)]
        self.i += 1
        return it


class K:
    pass


def bc(ap, n):
    return ap.unsqueeze(1).broadcast_to([ap.shape[0], n, ap.shape[1]])


def phase_mod(k, l):
    with Phase(k.nc, k.pool, "mod%d" % l) as ph:
        cv = ph.sb("cv", [128, 16, 2])
        s = ph.sb("s", [128, 16, 2])
        ph.ld(k.cols[:], k.d["cols"][l], "cols", ["cols"])
        ph.ld(cv[:], k.d["cvec"], "cv", ["cv"])
        ph.act(s[:], cv[:], AF.Silu, ["cv"], ["s"])
        wt = [ph.sb("w%d" % i, [128, 16, 1024]) for i in range(2)]
        psm = ph.ps("psm", [128, 96, 2])
        wsrc = k.d["w_mod"][l].rearrange("(kc p) f -> p kc f", p=128)
        for fb in range(12):
            w, wn = wt[fb % 2], "w%d" % (fb % 2)
            ph.ld(w[:, 0:8, :], wsrc[:, 0:8, fb * 1024:(fb + 1) * 1024], wn, [wn])
            ph.ld(w[:, 8:16, :], wsrc[:, 8:16, fb * 1024:(fb + 1) * 1024], wn, [wn])
            for fc in range(8):
                j = fb * 8 + fc
                for kc in range(16):
                    ph.mm(psm[:, j, :], w[:, kc, fc * 128:(fc + 1) * 128], s[:, kc, :], kc == 0, kc == 15, [wn, "s"], ["psm"])
        m = k.modsb
        for v in range(2):
            ph.tt("vector", m[:, :, v], psm[:, :, v], k.cols[:, C_BMOD:C_BMOD + 96], ALU.add, ["psm", "cols"], ["modsb"])
        for v in range(2):
            ph.stt(m[:, 16:32, v], m[:, 16:32, v], 1.0, k.cols[:, C_N1:C_N1 + 16], ALU.add, ALU.mult, ["modsb", "cols"], ["modsb"])
            ph.stt(m[:, 64:80, v], m[:, 64:80, v], 1.0, k.cols[:, C_N2:C_N2 + 16], ALU.add, ALU.mult, ["modsb", "cols"], ["modsb"])


def norm_tile(ph, k, xt, xtn, W, v, a_off, b_off, out, outn, tmp, tmpn, rs, rsn, psn, psnn, out_f32=None):
    ph.act(tmp[:, :, :W], xt[:, :, :W], AF.Square, [xtn], [tmpn])
    for c in range(16):
        ph.mm(psn[:, :W], k.c32[:, K_ONES:K_ONES + 128], tmp[:, c, :W], c == 0, c == 15, [tmpn, "c32"], [psnn])
    ph.act(rs[:, :W], psn[:, :W], AF.Sqrt, [psnn], [rsn], bias=k.epsc[:, 0:1], scale=1.0 / D)
    ph.recip(rs[:, :W], rs[:, :W], [rsn], [rsn])
    ph.tt("vector", tmp[:, :, :W], xt[:, :, :W], bc(rs[:, :W], 16), ALU.mult, [xtn, rsn], [tmpn])
    for c in range(16):
        ph.act(tmp[:, c, :W], tmp[:, c, :W], AF.Identity, [tmpn, "modsb"], [tmpn],
               bias=k.modsb[:, b_off + c, v:v + 1], scale=k.modsb[:, a_off + c, v:v + 1])
    ph.cp("gpsimd", out[:, :, :W], tmp[:, :, :W], [tmpn], [outn])


def phase_norm1(k, l, xsrc):
    with Phase(k.nc, k.pool, "n1_%d" % l) as ph:
        xts = [ph.sb("xt%d" % i, [128, 16, 512]) for i in range(2)]
        tmps = [ph.sb("tmp%d" % i, [128, 16, 512]) for i in range(2)]
        hbs = [ph.sb("hb%d" % i, [128, 16, 512], BF16) for i in range(2)]
        rss = [ph.sb("rs%d" % i, [128, 512]) for i in range(2)]
        psn = [ph.ps("psn%d" % i, [128, 512]) for i in range(2)]
        xv = xsrc.rearrange("(c p) t -> p c t", p=128)
        hv = k.d["hT"].rearrange("(c p) t -> p c t", p=128)
        tiles = TILES
        ph.ld(xts[0][:, :, :tiles[0][1]], xv[:, :, 0:tiles[0][1]], "xt0", ["xt0"])
        for i, (t0, W) in enumerate(tiles):
            b = i % 2
            if i + 1 < len(tiles):
                t1, W1 = tiles[i + 1]
                ph.ld(xts[1 - b][:, :, :W1], xv[:, :, t1:t1 + W1], "xt%d" % (1 - b), ["xt%d" % (1 - b)])
            norm_tile(ph, k, xts[b], "xt%d" % b, W, 1 if i == 0 else 0, 16, 0, hbs[b], "hb%d" % b,
                      tmps[b], "tmp%d" % b, rss[b], "rs%d" % b, psn[b], "psn%d" % b)
            ph.store(hv[:, :, t0:t0 + W], hbs[b][:, :, :W], "hb%d" % b, ["hb%d" % b])


def phase_proj(k, l):
    d = k.d
    with Phase(k.nc, k.pool, "pj%d" % l) as ph:
        wbs = [ph.sb("wb%d" % i, [128, 16, 1024], BF16) for i in range(2)]
        hts = [ph.sb("ht%d" % i, [128, 16, 512], BF16) for i in range(2)]
        css = [ph.sb("cs%d" % i, [128, 2, 512]) for i in range(2)]
        psA = Ring([(ph.ps("psA%d" % i, [128, 512]), "psA%d" % i) for i in range(3)])
        psN = (ph.ps("psN", [128, 512]), "psN")
        psR = (ph.ps("psR", [128, 512]), "psR")
        psV = (ph.ps("psV", [128, 256]), "psV")
        s32 = Ring([(ph.sb("s32_%d" % i, [128, 512]), "s32_%d" % i) for i in range(3)])
        s16 = Ring([(ph.sb("s16_%d" % i, [128, 512], BF16), "s16_%d" % i) for i in range(3)])
        sv = Ring([(ph.sb("sv_%d" % i, [128, 256], BF16), "sv_%d" % i) for i in range(2)])
        tq = Ring([tuple((ph.sb("tq%d_%d" % (i, j), [128, 512]), "tq%d_%d" % (i, j)) for j in range(5)) for i in range(2)])
        tg = Ring([tuple((ph.sb("tg%d_%d" % (i, j), [128, 512]), "tg%d_%d" % (i, j)) for j in range(2)) for i in range(2)])
        wsrc = d["w_in"][l].rearrange("(kc p) f -> p kc f", p=128)
        hv = d["hT"].rearrange("(c p) t -> p c t", p=128)
        groups = [(0, 1024), (1024, 1024), (2048, 1024), (3072, 1024), (4096, 16)]
        items = [(cg, ti) for cg in range(5) for ti in range(len(TILES))]
        ones32 = k.c32[:, K_ONES:K_ONES + 128]
        rm32 = k.c32[:, K_RM:K_RM + 128]
        cnt = [0]

        def load_w(cg):
            c0, n = groups[cg]
            wb, wn = wbs[cg % 2], "wb%d" % (cg % 2)
            for h in range(2):
                ph.ld(wb[:, 8 * h:8 * h + 8, :n], wsrc[:, 8 * h:8 * h + 8, c0:c0 + n], wn, [wn], eng="gpsimd")

        def load_item(ii):
            cg, ti = items[ii]
            t0, W = TILES[ti]
            b = ii % 2
            ph.ld(hts[b][:, :, :W], hv[:, :, t0:t0 + W], "ht%d" % b, ["ht%d" % b])
            if cg == 0:
                ph.ld(css[b][:, :, :W], d["rope"].rearrange("a p t -> p a t")[:, :, t0:t0 + W], "cs%d" % b, ["cs%d" % b])

        load_w(0)
        load_item(0)
        for ii, (cg, ti) in enumerate(items):
            t0, W = TILES[ti]
            b = ii % 2
            ht, htn = hts[b], "ht%d" % b
            wb, wn = wbs[cg % 2], "wb%d" % (cg % 2)
            if ti == 0 and cg + 1 < 5:
                load_w(cg + 1)
            if ii + 1 < len(items):
                load_item(ii + 1)
            nchunks = 1 if cg == 4 else 8
            for n in range(nchunks):
                if cg == 0 and n >= 6:
                    continue
                M = 16 if cg == 4 else 128
                ps, psn = psA.next()
                for kc in range(16):
                    ph.mm(ps[:M, :W], wb[:, kc, n * 128:n * 128 + M], ht[:, kc, :W], kc == 0, kc == 15, [wn, htn], [psn])
                cnt[0] += 1
                if cg == 0:
                    gcol = k.cols[:, C_QN:C_QN + 1] if n < 4 else k.cols[:, C_KN:C_KN + 1]
                    (xq, xqn), (sq, sqn), (rq, rqn), (xn, xnn), (t1, t1n) = tq.next()
                    cs, csn = css[b], "cs%d" % b
                    ph.cp("scalar", xq[:, :W], ps[:, :W], [psn], [xqn])
                    ph.act(sq[:, :W], ps[:, :W], AF.Square, [psn], [sqn])
                    ph.mm(psN[0][:, :W], ones32, sq[:, :W], True, True, [sqn, "c32"], [psN[1]])
                    ph.act(rq[:, :W], psN[0][:, :W], AF.Sqrt, [psN[1]], [rqn], bias=k.epsc[:, 0:1], scale=1.0 / 128)
                    ph.recip(rq[:, :W], rq[:, :W], [rqn], [rqn])
                    ph.stt(xn[:, :W], xq[:, :W], gcol, rq[:, :W], ALU.mult, ALU.mult, [xqn, rqn, "cols"], [xnn])
                    ph.mm(psR[0][:, :W], rm32, xn[:, :W], True, True, [xnn, "c32"], [psR[1]])
                    ph.tt("gpsimd", t1[:, :W], xn[:, :W], cs[:, 0, :W], ALU.mult, [xnn, csn], [t1n])
                    ph.tt("vector", sq[:, :W], psR[0][:, :W], cs[:, 1, :W], ALU.mult, [psR[1], csn], [sqn])
                    st, stn = s16.next()
                    ph.tt("vector", st[:, :W], t1[:, :W], sq[:, :W], ALU.add, [t1n, sqn], [stn])
                    dst = d["qT"][n * 128:(n + 1) * 128, t0:t0 + W] if n < 4 else d["kT"][(n - 4) * 128:(n - 3) * 128, t0:t0 + W]
                    ph.store(dst, st[:, :W], stn, [stn])
                elif cg == 1 and n >= 4:
                    (ta, tan), (tb, tbn) = tg.next()
                    ph.act(ta[:, :W], ps[:, :W], AF.Square, [psn], [tan])
                    ph.ts("vector", ta[:, :W], ta[:, :W], 0.044715, 1.0, ALU.mult, ALU.add, [tan], [tan])
                    ph.tt("vector", tb[:, :W], ta[:, :W], ps[:, :W], ALU.mult, [tan, psn], [tbn])
                    ph.act(tb[:, :W], tb[:, :W], AF.Sigmoid, [tbn], [tbn], scale=2.0 * math.sqrt(2.0 / math.pi))
                    st, stn = s32.next()
                    ph.tt("vector", st[:, :W], tb[:, :W], ps[:, :W], ALU.mult, [tbn, psn], [stn])
                    ph.store(d["lgT"][(n - 4) * 128:(n - 3) * 128, t0:t0 + W], st[:, :W], stn, [stn])
                elif cg == 2 and n < 4:
                    st, stn = s16.next()
                    ph.cp("vector" if cnt[0] % 2 else "scalar", st[:, :W], ps[:, :W], [psn], [stn])
                    ph.store(d["fT"][n * 128:(n + 1) * 128, t0:t0 + W], st[:, :W], stn, [stn])
                elif cg == 2:
                    st, stn = s32.next()
                    ph.act(st[:, :W], ps[:, :W], AF.Silu, [psn], [stn])
                    ph.store(d["zT"][(n - 4) * 128:(n - 3) * 128, t0:t0 + W], st[:, :W], stn, [stn])
                else:
                    st, stn = s32.next()
                    ph.cp("vector" if cnt[0] % 2 else "scalar", st[:M, :W], ps[:M, :W], [psn], [stn])
                    if cg == 1:
                        dst = d["lxT"][n * 128:(n + 1) * 128, t0:t0 + W]
                    elif cg == 3:
                        dst = d["xbcT"][n * 128:(n + 1) * 128, t0:t0 + W]
                    else:
                        dst = d["dtT"][:, t0:t0 + W]
                    ph.store(dst, st[:M, :W], stn, [stn])
            if cg == 0:
                for tc in range(W // 128):
                    for kc in range(16):
                        ph.mm(psV[0][:, :], ht[:, kc, tc * 128:(tc + 1) * 128], wb[:, kc, 768:1024], kc == 0, kc == 15, [wn, htn], [psV[1]])
                    st, stn = sv.next()
                    ph.cp("scalar", st[:, :], psV[0][:, :], [psV[1]], [stn])
                    ph.store(d["v"][t0 + tc * 128:t0 + (tc + 1) * 128, :], st[:, :], stn, [stn])


def phase_att(k, l):
    d = k.d
    scale = 128 ** -0.5
    with Phase(k.nc, k.pool, "att%d" % l) as ph:
        kT = ph.sb("kT", [128, 2, T], BF16)
        vs = ph.sb("vs", [128, NCH, 256], BF16)
        onesb = ph.sb("onesb", [128, 128], BF16)
        qs = [ph.sb("q%d" % i, [128, 4, 512], BF16) for i in range(2)]
        pts = Ring([(ph.sb("pt%d" % i, [128, 512], BF16), "pt%d" % i) for i in range(4)])
        psS = Ring([(ph.ps("psS%d" % i, [128, 512]), "psS%d" % i) for i in range(3)])
        psO = Ring([((ph.ps("psO%d" % i, [128, 512]), "psO%d" % i), (ph.ps("psL%d" % i, [128, 512]), "psL%d" % i)) for i in range(2)])
        rsb = Ring([(ph.sb("rsb%d" % i, [128, 512]), "rsb%d" % i) for i in range(2)])
        ost = Ring([(ph.sb("ost%d" % i, [128, 512], BF16), "ost%d" % i) for i in range(3)])
        ph.cp("vector", onesb[:], k.c32[:, K_ONES:K_ONES + 128], ["c32"], ["onesb"])
        kv = d["kT"].rearrange("(h p) t -> p h t", p=128)
        ph.ld(kT[:, 0, :], kv[:, 0, :], "kT", ["kT"])
        ph.ld(kT[:, 1, :], kv[:, 1, :], "kT", ["kT"])
        vv = d["v"].rearrange("(c p) e -> p c e", p=128)
        ph.ld(vs[:, 0:17, :], vv[:, 0:17, :], "vs", ["vs"])
        ph.ld(vs[:, 17:34, :], vv[:, 17:34, :], "vs", ["vs"])
        qv = d["qT"].rearrange("(h p) t -> p h t", p=128)
        ph.ld(qs[0][:, :, :256], qv[:, :, 0:256], "q0", ["q0"])
        for ti, (t0, W) in enumerate(TILES):
            b = ti % 2
            if ti + 1 < len(TILES):
                t1, W1 = TILES[ti + 1]
                ph.ld(qs[1 - b][:, :, :W1], qv[:, :, t1:t1 + W1], "q%d" % (1 - b), ["q%d" % (1 - b)])
            nsc = 2 if ti == 0 else NCH
            for h in range(4):
                kvh = h // 2
                (po, pon), (pl, pln) = psO.next()
                pend = {}

                def stA(sc, h=h, kvh=kvh, W=W, b=b):
                    ps, psn = psS.next()
                    ph.mm(ps[:, :W], kT[:, kvh, sc * 128:(sc + 1) * 128], qs[b][:, h, :W], True, True, ["kT", "q%d" % b], [psn])
                    pt, ptn = pts.next()
                    ph.act(pt[:, :W], ps[:, :W], AF.Exp, [psn], [ptn], scale=scale)
                    pend[sc] = (pt, ptn)

                def stB(sc, kvh=kvh, W=W, nsc=nsc, po=po, pon=pon, pl=pl, pln=pln):
                    pt, ptn = pend.pop(sc)
                    ph.mm(po[:, :W], vs[:, sc, kvh * 128:(kvh + 1) * 128], pt[:, :W], sc == 0, sc == nsc - 1, ["vs", ptn], [pon])
                    ph.mm(pl[:, :W], onesb[:], pt[:, :W], sc == 0, sc == nsc - 1, ["onesb", ptn], [pln])

                stA(0)
                if nsc > 1:
                    stA(1)
                for sc in range(nsc):
                    if sc + 2 < nsc:
                        stA(sc + 2)
                    stB(sc)
                rs, rsn = rsb.next()
                ph.recip(rs[:, :W], pl[:, :W], [pln], [rsn])
                st, stn = ost.next()
                ph.tt("vector", st[:, :W], po[:, :W], rs[:, :W], ALU.mult, [pon, rsn], [stn])
                ph.store(d["featT"][h * 128:(h + 1) * 128, t0:t0 + W], st[:, :W], stn, [stn])


def conv_seg(ph, k, u, un, x, xn, wcol0, bcol):
    ph.act(u[:, :], x[:, :], AF.Identity, [xn, "cols"], [un], bias=k.cols[:, bcol:bcol + 1], scale=k.cols[:, wcol0 + 2:wcol0 + 3])
    for j in (0, 1, 3):
        off = j - 2
        for (s0, s1) in ((0, NCTX), (NCTX, T)):
            lo, hi = max(s0, s0 - off), min(s1, s1 - off)
            ph.stt(u[:, lo:hi], x[:, lo + off:hi + off], k.cols[:, wcol0 + j:wcol0 + j + 1], u[:, lo:hi], ALU.mult, ALU.add, [xn, un, "cols"], [un])


def phase_lru(k, l):
    d = k.d
    with Phase(k.nc, k.pool, "lru%d" % l) as ph:
        x = ph.sb("x", [128, T])
        u = ph.sb("u", [128, T])
        lg = ph.sb("lg", [128, T])
        ra = ph.sb("ra", [128, T])
        ib = ph.sb("ib", [128, T])
        hf = ph.sb("hf", [128, T])
        hb = ph.sb("hb", [128, T])
        ob = ph.sb("ob", [128, T], BF16)
        wa = ph.sb("wa", [128, 2, 4, 128])
        wx = ph.sb("wx", [128, 2, 4, 128])
        sp = ph.sb("sp", [128, 8])
        psg = Ring([(ph.ps("psg%d" % i, [128, 512]), "psg%d" % i) for i in range(4)])
        ph.ld(wa[:], d["lru_wa"][l].rearrange("d n c e -> c d n e"), "wa", ["wa"])
        ph.ld(wx[:], d["lru_wx"][l].rearrange("d n c e -> c d n e"), "wx", ["wx"])
        ph.act(sp[:], k.cols[:, C_LLAM:C_LLAM + 8], AF.Exp, ["cols"], ["sp"], scale=-1.0)
        ph.act(sp[:], sp[:], AF.Ln, ["sp"], ["sp"], bias=k.onec[:, 0:1])
        ph.ts("vector", sp[:], sp[:], -8.0, None, ALU.mult, None, ["sp"], ["sp"])
        for n in range(4):
            ph.ld(x[:], d["lxT"][n * 128:(n + 1) * 128, :], "x", ["x"])
            ph.ld(lg[:], d["lgT"][n * 128:(n + 1) * 128, :], "lg", ["lg"])
            conv_seg(ph, k, u, "u", x, "x", C_LCW + 4 * n, C_LCB + n)
            for dr in range(2):
                for (t0, W) in TILES:
                    p1, p1n = psg.next()
                    ph.mm(p1[:, :W], wa[:, dr, n, :], u[:, t0:t0 + W], True, True, ["wa", "u"], [p1n])
                    ph.act(ra[:, t0:t0 + W], p1[:, :W], AF.Sigmoid, [p1n, "cols"], ["ra"], bias=k.cols[:, C_LBA + dr * 4 + n:C_LBA + dr * 4 + n + 1])
                    p2, p2n = psg.next()
                    ph.mm(p2[:, :W], wx[:, dr, n, :], u[:, t0:t0 + W], True, True, ["wx", "u"], [p2n])
                    ph.act(ib[:, t0:t0 + W], p2[:, :W], AF.Sigmoid, [p2n, "cols"], ["ib"], bias=k.cols[:, C_LBX + dr * 4 + n:C_LBX + dr * 4 + n + 1])
                ph.act(ra[:], ra[:], AF.Exp, ["ra", "sp"], ["ra"], scale=sp[:, dr * 4 + n:dr * 4 + n + 1])
                ph.act(x[:], ra[:], AF.Square, ["ra"], ["x"])
                ph.ts("vector", x[:], x[:], -1.0, 1.0, ALU.mult, ALU.add, ["x"], ["x"])
                ph.act(x[:], x[:], AF.Sqrt, ["x"], ["x"])
                ph.tt("gpsimd", ib[:], ib[:], u[:], ALU.mult, ["ib", "u"], ["ib"])
                ph.tt("vector", ib[:], ib[:], x[:], ALU.mult, ["ib", "x"], ["ib"])
                if dr == 0:
                    ph.scan(hf[:], ra[:], ib[:], 0.0, ["ra", "ib"], ["hf"])
                else:
                    ph.scan(hb[:, 0:NCTX][:, ::-1], ra[:, 0:NCTX][:, ::-1], ib[:, 0:NCTX][:, ::-1], 0.0, ["ra", "ib"], ["hb"])
                    ph.scan(hb[:, NCTX:T][:, ::-1], ra[:, NCTX:T][:, ::-1], ib[:, NCTX:T][:, ::-1], hb[:, 0:1], ["ra", "ib", "hb"], ["hb"])
            ph.tt("gpsimd", hf[:], hf[:], hb[:], ALU.add, ["hf", "hb"], ["hf"])
            ph.tt("vector", ob[:], hf[:], lg[:], ALU.mult, ["hf", "lg"], ["ob"])
            ph.store(d["featT"][512 + n * 128:512 + (n + 1) * 128, :], ob[:], "ob", ["ob"])


def phase_four1(k, l):
    d = k.d
    with Phase(k.nc, k.pool, "f1_%d" % l) as ph:
        fs = ph.sb("fs", [128, 4, T], BF16)
        cs = ph.sb("cs", [128, 256], BF16)
        pq = Ring([(ph.ps("pq%d" % i, [128, 4, 256]), "pq%d" % i) for i in range(3)])
        st = Ring([(ph.sb("st%d" % i, [128, 4, 256], BF16), "st%d" % i) for i in range(3)])
        ph.ld(cs[:], d["cs128"], "cs", ["cs"])
        fv = d["fT"].rearrange("(g p) t -> p g t", p=128)
        for g in range(4):
            ph.ld(fs[:, g, :], fv[:, g, :], "fs", ["fs"])
        pv = d["PQ"].rearrange("(c p) g m -> p c g m", p=128)
        for tc in range(NCH):
            p, pn = pq.next()
            for g in range(4):
                ph.mm(p[:, g, :], fs[:, g, tc * 128:(tc + 1) * 128], cs[:], True, True, ["fs", "cs"], [pn])
            s, sn = st.next()
            ph.cp("vector" if tc % 2 else "scalar", s[:], p[:], [pn], [sn])
            ph.store(pv[:, tc, :, :], s[:], sn, [sn])


def phase_four2(k, l):
    d = k.d
    with Phase(k.nc, k.pool, "f2_%d" % l) as ph:
        PQ = ph.sb("PQ", [128, NCH, 4, 256], BF16)
        dfc = ph.sb("dfc", [128, 2, 2, 256], BF16)
        units = Ring([(ph.sb("du%d" % i, [128, 32, 512], BF16), "du%d" % i) for i in range(3)])
        psy = [[(ph.ps("py%d_%d" % (s, g), [128, 512]), "py%d_%d" % (s, g)) for g in range(4)] for s in range(2)]
        st = Ring([(ph.sb("st%d" % i, [128, 512], BF16), "st%d" % i) for i in range(4)])
        pv = d["PQ"].rearrange("(c p) g m -> p c g m", p=128)
        ph.ld(PQ[:, 0:17], pv[:, 0:17], "PQ", ["PQ"])
        ph.ld(PQ[:, 17:34], pv[:, 17:34], "PQ", ["PQ"])
        ph.ld(dfc[:], d["dftc"].rearrange("m (c p) q -> p m c q", p=128), "dfc", ["dfc"])
        ul = [(kt, m) for kt in range(8) for m in range(2)]
        dv = [d["dftL"][m].rearrange("(c p) q -> p c q", p=128) for m in range(2)]
        cur = {}

        def load_u(i):
            kt, m = ul[i]
            u, un = units.next()
            ph.ld(u[:, 0:16, :], dv[m][:, 0:16, kt * 512:(kt + 1) * 512], un, [un])
            ph.ld(u[:, 16:32, :], dv[m][:, 16:32, kt * 512:(kt + 1) * 512], un, [un])
            cur[i] = (u, un)

        load_u(0)
        load_u(1)
        for g in range(4):
            p, pn = psy[1][g]
            for m in range(2):
                for c in range(2):
                    ph.mm(p[:, :256], PQ[:, c, g, m * 128:(m + 1) * 128], dfc[:, m, c, :], m == 0 and c == 0, m == 1 and c == 1, ["PQ", "dfc"], [pn])
            s, sn = st.next()
            ph.cp("vector", s[:, :256], p[:, :256], [pn], [sn])
            ph.store(d["featT"][1024 + g * 128:1024 + (g + 1) * 128, 0:256], s[:, :256], sn, [sn])
        for i, (kt, m) in enumerate(ul):
            if i + 2 < len(ul):
                load_u(i + 2)
            u, un = cur.pop(i)
            for g in range(4):
                p, pn = psy[kt % 2][g]
                for tc in range(32):
                    ph.mm(p[:, :], PQ[:, 2 + tc, g, m * 128:(m + 1) * 128], u[:, tc, :], m == 0 and tc == 0, m == 1 and tc == 31, ["PQ", un], [pn])
                if m == 1:
                    s, sn = st.next()
                    ph.cp("vector" if g % 2 else "scalar", s[:, :], p[:, :], [pn], [sn])
                    ph.store(d["featT"][1024 + g * 128:1024 + (g + 1) * 128, 256 + kt * 512:256 + (kt + 1) * 512], s[:, :], sn, [sn])


def phase_ssd_a(k, l):
    d = k.d
    with Phase(k.nc, k.pool, "sa%d" % l) as ph:
        x = ph.sb("x", [128, T])
        u = ph.sb("u", [128, T])
        ub = ph.sb("ub", [128, T], BF16)
        xtm = ph.sb("xtm", [128, NCH, 512], BF16)
        idb = ph.sb("idb", [128, 128], BF16)
        dt = ph.sb("dt", [64, T])
        y = ph.sb("y", [64, T])
        e = ph.sb("e", [64, T])
        P = ph.sb("P", [64, T])
        ones = ph.sb("ones", [64, T])
        acol = ph.sb("acol", [64, 1])
        bcs = ph.sb("bcs", [128, NCH, 40])
        pst = Ring([(ph.ps("pst%d" % i, [128, 4, 128], BF16), "pst%d" % i) for i in range(2)])
        psb = Ring([(ph.ps("psb%d" % i, [128, 8, 40]), "psb%d" % i) for i in range(2)])
        ph.cp("vector", idb[:], k.c32[:, K_ID:K_ID + 128], ["c32"], ["idb"])
        ph.memset("gpsimd", dt[:], 0.0, ["dt"])
        ph.memset("gpsimd", P[:], 0.0, ["P"])
        ph.memset("gpsimd", ones[:], 1.0, ["ones"])
        ph.ld(dt[0:8, :], d["dtT"][0:8, :], "dt", ["dt"], r=["dt"])
        ph.ld(dt[32:40, :], d["dtT"][8:16, :], "dt", ["dt"], r=["dt"])
        ph.act(y[0:40, :], dt[0:40, :], AF.Identity, ["dt", "cols"], ["y"], bias=k.cols[0:40, C_DTB:C_DTB + 1])
        ph.act(e[0:40, :], y[0:40, :], AF.Abs, ["y"], ["e"])
        ph.act(e[0:40, :], e[0:40, :], AF.Exp, ["e"], ["e"], scale=-1.0)
        ph.act(e[0:40, :], e[0:40, :], AF.Ln, ["e"], ["e"], bias=k.onec[0:40, 0:1])
        ph.ts("vector", y[0:40, :], y[0:40, :], 0.0, None, ALU.max, None, ["y"], ["y"])
        ph.tt("vector", dt[0:40, :], y[0:40, :], e[0:40, :], ALU.add, ["y", "e"], ["dt"])
        ph.act(e[0:40, :], dt[0:40, :], AF.Ln, ["dt"], ["e"])
        ph.act(acol[0:40, :], k.cols[0:40, C_ALOG:C_ALOG + 1], AF.Exp, ["cols"], ["acol"])
        ph.ts("vector", acol[0:40, :], acol[0:40, :], -1.0, None, ALU.mult, None, ["acol"], ["acol"])
        ph.ts("vector", y[0:40, :], dt[0:40, :], acol[0:40, 0:1], None, ALU.mult, None, ["dt", "acol"], ["y"])
        ph.scan(P[0:8, :], ones[0:8, :], y[0:8, :], 0.0, ["ones", "y", "P"], ["P"])
        ph.scan(P[32:40, NCTX:T], ones[32:40, NCTX:T], y[32:40, NCTX:T], 0.0, ["ones", "y", "P"], ["P"])
        ph.scan(P[32:40, 0:NCTX], ones[32:40, 0:NCTX], y[32:40, 0:NCTX], P[32:40, T - 1:T], ["ones", "y", "P"], ["P"])
        ph.tt("vector", P[32:40, :], y[32:40, :], P[32:40, :], ALU.subtract, ["y", "P"], ["P"])
        ph.store(d["Rrow"], P[0:40, :], "P", ["P"])
        ph.tt("vector", e[0:40, :], e[0:40, :], P[0:40, :], ALU.subtract, ["e", "P"], ["e"])
        for c8 in range(0, NCH, 8):
            nb = min(8, NCH - c8)
            p, pn = psb.next()
            for j in range(nb):
                tc = c8 + j
                ph.tr(p[:, j, :], e[0:40, tc * 128:(tc + 1) * 128], k.c32[0:40, K_ID:K_ID + 40], ["e", "c32"], [pn])
            ph.cp("vector", bcs[:, c8:c8 + nb, :], p[:, 0:nb, :], [pn], ["bcs"])
        ph.store(d["bcol"].rearrange("(c p) r -> p c r", p=128), bcs[:], "bcs", ["bcs"])
        for c in range(8):
            ph.ld(x[:], d["xbcT"][c * 128:(c + 1) * 128, :], "x", ["x"])
            conv_seg(ph, k, u, "u", x, "x", C_SCW + 4 * c, C_SCB + c)
            ph.act(u[:], u[:], AF.Silu, ["u"], ["u"])
            if c < 4:
                ph.store(d["xsT"][c * 128:(c + 1) * 128, :], u[:], "u", ["u"])
            ph.cp("gpsimd", ub[:], u[:], ["u"], ["ub"])
            if c < 4:
                for c4 in range(0, NCH, 4):
                    nb = min(4, NCH - c4)
                    p, pn = pst.next()
                    for j in range(nb):
                        tc = c4 + j
                        ph.tr(p[:, j, :], ub[:, tc * 128:(tc + 1) * 128], idb[:], ["ub", "idb"], [pn])
                    ph.cp("vector" if (c4 // 4) % 2 else "scalar", xtm[:, c4:c4 + nb, c * 128:(c + 1) * 128], p[:, 0:nb, :], [pn], ["xtm"])
            elif c < 6:
                ph.store(d["BT"][c - 4], ub[:], "ub", ["ub"])
            else:
                ph.store(d["CT"][c - 6], ub[:], "ub", ["ub"])
        ph.store(d["x_tm"].rearrange("(c p) f -> p c f", p=128), xtm[:], "xtm", ["xtm"])


def ssd_pairs(ti):
    t0, W = TILES[ti]
    prs = []
    if ti == 0:
        for dr in range(2):
            for kk in range(2):
                prs.append((dr, kk, dr * 4 + kk))
        return prs
    c0 = t0 // 128
    for sc in range(c0):
        prs.append((0, sc, None))
    for kk in range(4):
        prs.append((0, c0 + kk, kk))
    for kk in range(4):
        prs.append((1, c0 + kk, 4 + kk))
    for sc in range(c0 + 4, NCH):
        prs.append((1, sc, None))
    for sc in range(2):
        prs.append((1, sc, None))
    return prs


def phase_ssd_b(k, l):
    d = k.d
    with Phase(k.nc, k.pool, "sb%d" % l) as ph:
        BT = ph.sb("BT", [128, 2, T], BF16)
        CT = ph.sb("CT", [128, 2, T], BF16)
        xtm = ph.sb("xtm", [128, NCH, 512], BF16)
        R = ph.sb("R", [40, T])
        bcs = ph.sb("bcs", [128, NCH, 40])
        mk = ph.sb("mk", [128, 8, 512])
        sel = ph.sb("sel", [40, 16 * 128])
        ph.ld(sel[:], d["c32"][0:40, K_SEL:NK], "sel", ["sel"])
        xs = [ph.sb("xs%d" % i, [128, 4, 512]) for i in range(2)]
        zs = [ph.sb("zs%d" % i, [128, 4, 512]) for i in range(2)]
        yz = ph.sb("yz", [128, 4, 512])
        sq = Ring([(ph.sb("sq%d" % i, [128, 512]), "sq%d" % i) for i in range(2)])
        rs = ph.sb("rs", [128, 512])
        Lt = Ring([(ph.sb("Lt%d" % i, [128, 512]), "Lt%d" % i) for i in range(4)])
        tm = Ring([(ph.sb("tm%d" % i, [128, 512]), "tm%d" % i) for i in range(2)])
        Mt = Ring([(ph.sb("Mt%d" % i, [128, 512], BF16), "Mt%d" % i) for i in range(4)])
        ost = Ring([(ph.sb("ost%d" % i, [128, 512], BF16), "ost%d" % i) for i in range(3)])
        psY = Ring([(ph.ps("psY%d" % i, [128, 512]), "psY%d" % i) for i in range(2)])
        psRr = [(ph.ps("psR%d" % i, [128, 512]), "psR%d" % i) for i in range(2)]
        psG = Ring([(ph.ps("psG%d" % i, [128, 512]), "psG%d" % i) for i in range(3)])
        psN = (ph.ps("psN", [128, 512]), "psN")
        for g in range(2):
            ph.ld(BT[:, g, :], d["BT"][g], "BT", ["BT"])
            ph.ld(CT[:, g, :], d["CT"][g], "CT", ["CT"])
        xv = d["x_tm"].rearrange("(c p) f -> p c f", p=128)
        ph.ld(xtm[:, 0:17], xv[:, 0:17], "xtm", ["xtm"])
        ph.ld(xtm[:, 17:34], xv[:, 17:34], "xtm", ["xtm"])
        ph.ld(R[:], d["Rrow"], "R", ["R"])
        ph.ld(bcs[:], d["bcol"].rearrange("(c p) r -> p c r", p=128), "bcs", ["bcs"])
        ph.ld(mk[:], d["masks"].rearrange("m p t -> p m t"), "mk", ["mk"])
        xsv = d["xsT"].rearrange("(c p) t -> p c t", p=128)
        zv = d["zT"].rearrange("(c p) t -> p c t", p=128)

        def load_t(ti):
            t0, W = TILES[ti]
            b = ti % 2
            ph.ld(xs[b][:, :, :W], xsv[:, :, t0:t0 + W], "xs%d" % b, ["xs%d" % b])
            ph.ld(zs[b][:, :, :W], zv[:, :, t0:t0 + W], "zs%d" % b, ["zs%d" % b])

        load_t(0)
        ones32 = k.c32[:, K_ONES:K_ONES + 128]
        for ti, (t0, W) in enumerate(TILES):
            b = ti % 2
            if ti + 1 < len(TILES):
                load_t(ti + 1)
            prs = ssd_pairs(ti)
            for cch in range(4):
                g = cch // 2
                py, pyn = psY.next()
                for hh in range(2):
                    h = cch * 2 + hh
                    for dr in range(2):
                        j = dr * 8 + h
                        ph.mm(psRr[dr][0][:, :W], sel[0:40, j * 128:(j + 1) * 128], R[0:40, t0:t0 + W], True, True, ["sel", "R"], [psRr[dr][1]])
                    pend = {}

                    def stA(pi, h=h, g=g, W=W, t0=t0):
                        dr, sc, mi = prs[pi]
                        row = h if dr == 0 else 32 + h
                        pg, pgn = psG.next()
                        ph.mm(pg[:, :W], BT[:, g, sc * 128:(sc + 1) * 128], CT[:, g, t0:t0 + W], True, True, ["BT", "CT"], [pgn])
                        bcol = bcs[:, sc, row:row + 1]
                        L, Ln_ = Lt.next()
                        if mi is None:
                            ph.act(L[:, :W], psRr[dr][0][:, :W], AF.Exp, [psRr[dr][1], "bcs"], [Ln_], bias=bcol)
                        else:
                            t_, tn = tm.next()
                            ph.stt(t_[:, :W], psRr[dr][0][:, :W], bcol, mk[:, mi, :W], ALU.add, ALU.add, [psRr[dr][1], "bcs", "mk"], [tn])
                            ph.act(L[:, :W], t_[:, :W], AF.Exp, [tn], [Ln_])
                        M, Mn = Mt.next()
                        ph.tt("vector", M[:, :W], pg[:, :W], L[:, :W], ALU.mult, [pgn, Ln_], [Mn])
                        pend[pi] = (M, Mn, sc)

                    def stB(pi, h=h, hh=hh, W=W, py=py, pyn=pyn):
                        M, Mn, sc = pend.pop(pi)
                        ph.mm(py[hh * 64:(hh + 1) * 64, :W], xtm[:, sc, h * 64:(h + 1) * 64], M[:, :W], pi == 0, pi == len(prs) - 1, ["xtm", Mn], [pyn])

                    stA(0)
                    if len(prs) > 1:
                        stA(1)
                    for pi in range(len(prs)):
                        if pi + 2 < len(prs):
                            stA(pi + 2)
                        stB(pi)
                s_, sn = sq.next()
                ph.stt(s_[:, :W], xs[b][:, cch, :W], k.cols[:, C_SD + cch:C_SD + cch + 1], py[:, :W], ALU.mult, ALU.add, ["xs%d" % b, "cols", pyn], [sn])
                ph.tt("gpsimd", yz[:, cch, :W], s_[:, :W], zs[b][:, cch, :W], ALU.mult, [sn, "zs%d" % b], ["yz%d" % cch])
                ph.act(s_[:, :W], yz[:, cch, :W], AF.Square, ["yz%d" % cch], [sn])
                ph.mm(psN[0][:, :W], ones32, s_[:, :W], cch == 0, cch == 3, ["c32", sn], [psN[1]])
            ph.act(rs[:, :W], psN[0][:, :W], AF.Sqrt, [psN[1]], ["rs"], bias=k.epsc[:, 0:1], scale=1.0 / 512)
            ph.recip(rs[:, :W], rs[:, :W], ["rs"], ["rs"])
            for cch in range(4):
                o, on = ost.next()
                ph.stt(o[:, :W], yz[:, cch, :W], k.cols[:, C_SN + cch:C_SN + cch + 1], rs[:, :W], ALU.mult, ALU.mult, ["yz%d" % cch, "cols", "rs"], [on])
                ph.store(d["featT"][1536 + cch * 128:1536 + (cch + 1) * 128, t0:t0 + W], o[:, :W], on, [on])


def phase_out(k, l, xsrc, tiles):
    d = k.d
    with Phase(k.nc, k.pool, "out%d" % l) as ph:
        wo = ph.sb("wo", [128, 16, D], BF16)
        fts = [ph.sb("ft0", [128, 16, 512], BF16)] * 2
        xts = [ph.sb("xt%d" % i, [128, 16, 512]) for i in range(2)]
        tmp = ph.sb("tmp", [128, 16, 512])
        rs = ph.sb("rs", [128, 512])
        rw = ph.sb("rw", [128, 16, NE])
        rb = ph.sb("rb", [128, NE])
        lgt = Ring([tuple((ph.sb("lg%d_%d" % (i, j), [128, NE]), "lg%d_%d" % (i, j)) for j in range(3)) for i in range(2)])
        m8 = Ring([(ph.sb("m8_%d" % i, [128, 8]), "m8_%d" % i) for i in range(2)])
        sm = Ring([(ph.sb("sm%d" % i, [128, 2]), "sm%d" % i) for i in range(2)])
        gts = ph.sb("gts", [NE, 512])
        psA = Ring([(ph.ps("psA%d" % i, [128, 512]), "psA%d" % i) for i in range(4)])
        psn = (ph.ps("psn", [128, 512]), "psn")
        psl = Ring([(ph.ps("psl%d" % i, [128, NE]), "psl%d" % i) for i in range(2)])
        pst = (ph.ps("pst", [NE, 512]), "pst")
        wsrc = d["w_out"][l].rearrange("(kc p) f -> p kc f", p=128)
        for q4 in range(4):
            ph.ld(wo[:, 4 * q4:4 * q4 + 4, :], wsrc[:, 4 * q4:4 * q4 + 4, :], "wo", ["wo"], eng="gpsimd")
        ph.ld(rw[:], d["router_w"][l].rearrange("(kc p) e -> p kc e", p=128), "rw", ["rw"])
        ph.ld(rb[:], d["rb"][l], "rb", ["rb"])
        fv = d["featT"].rearrange("(c p) t -> p c t", p=128)
        xv = xsrc.rearrange("(c p) t -> p c t", p=128)
        xo = d["xw"].rearrange("(c p) t -> p c t", p=128)
        hv = d["h2T"].rearrange("(c p) t -> p c t", p=128)

        def load_t(i):
            t0, W = tiles[i]
            b = i % 2
            ph.ld(xts[b][:, :, :W], xv[:, :, t0:t0 + W], "xt%d" % b, ["xt%d" % b])

        load_t(0)
        for i, (t0, W) in enumerate(tiles):
            b = i % 2
            v = 1 if t0 == 0 else 0
            ph.ld(fts[b][:, :, :W], fv[:, :, t0:t0 + W], "ft0", ["ft0"])
            if i + 1 < len(tiles):
                load_t(i + 1)
            ft, ftn, xt, xtn = fts[b], "ft0", xts[b], "xt%d" % b
            for dc in range(16):
                p, pn = psA.next()
                for kc in range(16):
                    ph.mm(p[:, :W], wo[:, kc, dc * 128:(dc + 1) * 128], ft[:, kc, :W], kc == 0, kc == 15, ["wo", ftn], [pn])
                ph.stt(xt[:, dc, :W], p[:, :W], k.modsb[:, 32 + dc, v:v + 1], xt[:, dc, :W], ALU.mult, ALU.add, [pn, "modsb", xtn], [xtn])
            ph.store(xo[:, :, t0:t0 + W], xt[:, :, :W], xtn, [xtn])
            norm_tile(ph, k, xt, xtn, W, v, 64, 48, ft, "ft0", tmp, "tmp", rs, "rs", psn[0], psn[1])
            ph.store(hv[:, :, t0:t0 + W], ft[:, :, :W], "ft0", ["ft0"])
            for tc in range(W // 128):
                pl, pln = psl.next()
                for kc in range(16):
                    ph.mm(pl[:, :], tmp[:, kc, tc * 128:(tc + 1) * 128], rw[:, kc, :], kc == 0, kc == 15, ["tmp", "rw"], [pln])
                (lg, lgn), (ex, exn), (mkk, mkn) = lgt.next()
                m, mn = m8.next()
                s2, s2n = sm.next()
                ph.tt("vector", lg[:], pl[:], rb[:], ALU.add, [pln, "rb"], [lgn])
                ph.op("vector", lambda e, m=m, lg=lg: e.max(out=m[:], in_=lg[:]), [lgn], [mn])
                ph.ts("vector", mkk[:], lg[:], m[:, 3:4], None, ALU.is_ge, None, [lgn, mn], [mkn])
                ph.ts("vector", s2[:, 0:1], m[:, 0:1], -1.0, None, ALU.mult, None, [mn], [s2n])
                ph.act(ex[:], lg[:], AF.Exp, [lgn, s2n], [exn], bias=s2[:, 0:1])
                ph.tt("vector", ex[:], ex[:], mkk[:], ALU.mult, [exn, mkn], [exn])
                ph.op("vector", lambda e, s2=s2, ex=ex: e.reduce_sum(out=s2[:, 1:2], in_=ex[:], axis=AX.X), [exn], [s2n])
                ph.recip(s2[:, 1:2], s2[:, 1:2], [s2n], [s2n])
                ph.ts("vector", ex[:], ex[:], s2[:, 1:2], None, ALU.mult, None, [exn, s2n], [exn])
                ph.tr(pst[0][:, tc * 128:(tc + 1) * 128], ex[:], k.c32[:, K_ID:K_ID + 128], [exn, "c32"], [pst[1]])
            ph.cp("vector", gts[:, :W], pst[0][:, :W], [pst[1]], ["gts"])
            ph.store(d["gatesT"][:, t0:t0 + W], gts[:, :W], "gts", ["gts"])


def phase_cast(k, l):
    d = k.d
    with Phase(k.nc, k.pool, "cast%d" % l) as ph:
        for e in range(NE):
            src = d["exp_w_gu"][l, e].rearrange("k (j two) -> k two j", two=2)
            for h in range(2):
                for q in range(4):
                    ph.dma("gpsimd", lambda en, e=e, h=h, q=q, src=src: en.dma_start(
                        out=d["wgu16"][e, h, q * 512:(q + 1) * 512, :], in_=src[q * 512:(q + 1) * 512, h, :],
                        allow_slow_non_contiguous=True), "cast%d" % ((e * 8 + h * 4 + q) % 8))
            for q in range(2):
                ph.dma("gpsimd", lambda en, e=e, q=q: en.dma_start(
                    out=d["wdn16"][e, q * 384:(q + 1) * 384, :], in_=d["exp_w_dn"][l, e, q * 384:(q + 1) * 384, :]),
                    "castd%d" % ((e * 2 + q) % 4))


def phase_moe(k, l, supers):
    d = k.d
    with Phase(k.nc, k.pool, "moe%d" % l) as ph:
        h2 = ph.sb("h2", [128, 16, 1024], BF16)
        acc = ph.sb("acc", [128, 16, 1024])
        wg = [ph.sb("wg%d" % i, [128, 16, 512], BF16) for i in range(2)]
        wd = [ph.sb("wd%d" % i, [128, 2, D], BF16) for i in range(2)]
        Gb = [ph.sb("Gb%d" % i, [128, 1024]) for i in range(2)]
        bdn = ph.sb("bdn", [NE, D])
        gT = ph.sb("gT", [NE, 1024])
        tmps = Ring([tuple((ph.sb("t%d_%d" % (i, j), [128, 512]), "t%d_%d" % (i, j)) for j in range(4)) for i in range(2)])
        actT = Ring([(ph.sb("a%d" % i, [128, 2, 512], BF16), "a%d" % i) for i in range(3)])
        xc = Ring([(ph.sb("xc%d" % i, [128, 512]), "xc%d" % i) for i in range(3)])
        psg = Ring([(ph.ps("psg%d" % i, [128, 512]), "psg%d" % i) for i in range(2)])
        psu = Ring([(ph.ps("psu%d" % i, [128, 512]), "psu%d" % i) for i in range(2)])
        psd = Ring([(ph.ps("psd%d" % i, [128, 512]), "psd%d" % i) for i in range(4)])
        bu1 = ph.sb("bu1", [128, 192])
        ph.ts("vector", bu1[:], k.cols[:, C_BU:C_BU + 192], 1.0, None, ALU.add, None, ["cols"], ["bu1"])
        ph.ld(bdn[:], d["exp_b_dn"][l], "bdn", ["bdn"])
        hv = d["h2T"].rearrange("(c p) t -> p c t", p=128)
        xw = d["xw"].rearrange("(c p) t -> p c t", p=128)
        units = [(e, jp) for e in range(NE) for jp in range(3)]

        for (s0, Ws) in supers:
            tts = [(o, min(512, Ws - o)) for o in range(0, Ws, 512)]
            ph.ld(h2[:, 0:8, :Ws], hv[:, 0:8, s0:s0 + Ws], "h2", ["h2"])
            ph.ld(h2[:, 8:16, :Ws], hv[:, 8:16, s0:s0 + Ws], "h2", ["h2"])
            ph.ld(gT[:, :Ws], d["gatesT"][:, s0:s0 + Ws], "gT", ["gT"])
            for (o, W) in tts:
                for dc in range(16):
                    p, pn = psd.next()
                    ph.mm(p[:, :W], bdn[:, dc * 128:(dc + 1) * 128], gT[:, o:o + W], True, True, ["bdn", "gT"], [pn])
                    ph.cp("scalar", acc[:, dc, o:o + W], p[:, :W], [pn], ["acc%d" % dc])

            def load_unit(ui):
                e, jp = units[ui]
                b = ui % 2
                src = d["exp_w_gu"][l, e].rearrange("(kc p) f -> p kc f", p=128)
                ph.ld(wg[b][:, 0:8, :], src[:, 0:8, jp * 512:(jp + 1) * 512], "wg%d" % b, ["wg%d" % b], eng="gpsimd")
                ph.ld(wg[b][:, 8:16, :], src[:, 8:16, jp * 512:(jp + 1) * 512], "wg%d" % b, ["wg%d" % b], eng="gpsimd")
                ph.ld(wd[b][:], d["exp_w_dn"][l, e].rearrange("(j p) f -> p j f", p=128)[:, 2 * jp:2 * jp + 2, :], "wd%d" % b, ["wd%d" % b], eng="gpsimd")
                if jp == 0:
                    ph.ld(Gb[e % 2][:, :Ws], d["gatesT"][e, s0:s0 + Ws].partition_broadcast(128), "Gb%d" % (e % 2), ["Gb%d" % (e % 2)])

            items = [(ui, ti) for ui in range(len(units)) for ti in range(len(tts))]
            pend = {}

            def stGU(idx):
                ui, ti = items[idx]
                e, jp = units[ui]
                o, W = tts[ti]
                b = ui % 2
                wgb, wgn = wg[b], "wg%d" % b
                gb, gbn = Gb[e % 2], "Gb%d" % (e % 2)
                at, atn = actT.next()
                for jl in range(2):
                    jc = 2 * jp + jl
                    pg, pgn = psg.next()
                    pu, pun = psu.next()
                    for kc in range(16):
                        ph.mm(pg[:, :W], wgb[:, kc, jl * 256:(jl + 1) * 256:2], h2[:, kc, o:o + W], kc == 0, kc == 15, [wgn, "h2"], [pgn])
                        if kc % 4 == 3:
                            yield
                    for kc in range(16):
                        ph.mm(pu[:, :W], wgb[:, kc, jl * 256 + 1:(jl + 1) * 256:2], h2[:, kc, o:o + W], kc == 0, kc == 15, [wgn, "h2"], [pun])
                        if kc % 4 == 3:
                            yield
                    (g1, g1n), (sg, sgn), (u1, u1n), (t1, t1n) = tmps.next()
                    cb = e * 6 + jc
                    ph.ts("vector", g1[:, :W], pg[:, :W], k.cols[:, C_BG + cb:C_BG + cb + 1], 7.0, ALU.add, ALU.min, [pgn, "cols"], [g1n])
                    ph.act(sg[:, :W], g1[:, :W], AF.Sigmoid, [g1n], [sgn], scale=1.702)
                    ph.ts("vector", u1[:, :W], pu[:, :W], bu1[:, cb:cb + 1], 8.0, ALU.add, ALU.min, [pun, "bu1"], [u1n])
                    ph.stt(t1[:, :W], u1[:, :W], -6.0, g1[:, :W], ALU.max, ALU.mult, [u1n, g1n], [t1n])
                    ph.tt("gpsimd", t1[:, :W], t1[:, :W], sg[:, :W], ALU.mult, [t1n, sgn], [t1n])
                    ph.tt("gpsimd", at[:, jl, :W], t1[:, :W], gb[:, o:o + W], ALU.mult, [t1n, gbn], [atn])
                pend[idx] = (at, atn)

            def stD(idx):
                ui, ti = items[idx]
                o, W = tts[ti]
                b = ui % 2
                wdb, wdn = wd[b], "wd%d" % b
                at, atn = pend.pop(idx)
                for dc in range(16):
                    p, pn = psd.next()
                    for jl in range(2):
                        ph.mm(p[:, :W], wdb[:, jl, dc * 128:(dc + 1) * 128], at[:, jl, :W], jl == 0, jl == 1, [wdn, atn], [pn])
                    ph.tt("vector", acc[:, dc, o:o + W], acc[:, dc, o:o + W], p[:, :W], ALU.add, ["acc%d" % dc, pn], ["acc%d" % dc])
                    yield

            load_unit(0)
            load_unit(1)
            for _ in stGU(0):
                pass
            for idx in range(len(items)):
                gens = [stD(idx)] + ([stGU(idx + 1)] if idx + 1 < len(items) else [])
                while gens:
                    for g_ in list(gens):
                        try:
                            next(g_)
                        except StopIteration:
                            gens.remove(g_)
                ui, ti = items[idx]
                if ti == len(tts) - 1 and ui + 2 < len(units):
                    load_unit(ui + 2)
            for (o, W) in tts:
                a0, a1 = s0 + o, s0 + o + W
                segs = [(0, W, 0)] if a0 >= NCTX else ([(0, W, 1)] if a1 <= NCTX else [(0, NCTX - a0, 1), (NCTX - a0, W, 0)])
                for dc in range(16):
                    x_, xn_ = xc.next()
                    ph.ld(x_[:, :W], xw[:, dc, a0:a1], xn_, [xn_])
                    for (c0, c1, v) in segs:
                        ph.stt(x_[:, c0:c1], acc[:, dc, o + c0:o + c1], k.modsb[:, 80 + dc, v:v + 1], x_[:, c0:c1], ALU.mult, ALU.add, ["acc%d" % dc, "modsb", xn_], [xn_])
                    ph.store(xw[:, dc, a0:a1], x_[:, :W], xn_, [xn_])


IN_SPECS = [
    ("xT", [D, T], F32), ("cvec", [128, 16, 2], F32), ("cols", [DEPTH, 128, NCOLS], F32), ("rb", [DEPTH, 128, NE], F32),
    ("w_mod", [DEPTH, D, 6 * D], F32), ("w_in", [DEPTH, D, D_IN], F32), ("w_out", [DEPTH, D, D], F32),
    ("lru_wa", [DEPTH, 2, 4, 128, 128], F32), ("lru_wx", [DEPTH, 2, 4, 128, 128], F32),
    ("router_w", [DEPTH, D, NE], F32), ("exp_w_gu", [DEPTH, NE, D, 2 * EFF], F32), ("exp_w_dn", [DEPTH, NE, EFF, D], F32),
    ("exp_b_dn", [DEPTH, NE, D], F32),
    ("c32", [128, NK], F32), ("masks", [8, 128, 512], F32), ("rope", [2, 128, T], F32),
    ("cs128", [128, 256], BF16), ("dftL", [2, SEQ, SEQ], BF16), ("dftc", [2, NCTX, NCTX], BF16),
]
SCRATCH = [
    ("hT", [D, T], BF16), ("qT", [512, T], BF16), ("kT", [256, T], BF16), ("v", [T, 256], BF16),
    ("lxT", [512, T], F32), ("lgT", [512, T], F32), ("fT", [512, T], BF16), ("zT", [512, T], F32),
    ("xbcT", [1024, T], F32), ("dtT", [16, T], F32), ("featT", [D, T], BF16), ("PQ", [T, 4, 256], BF16),
    ("xsT", [512, T], F32), ("x_tm", [T, 512], BF16), ("BT", [2, 128, T], BF16), ("CT", [2, 128, T], BF16),
    ("Rrow", [40, T], F32), ("bcol", [T, 40], F32), ("h2T", [D, T], BF16), ("gatesT", [NE, T], F32),
]


def build_program(layers=range(DEPTH), phases=None, dbg=()):
    nc = bass.Bass("TRN2", target_bir_lowering=False)
    k = K()
    k.nc = nc
    k.d = {}
    for name, shape, dt in IN_SPECS:
        k.d[name] = nc.dram_tensor(name, shape, dt, kind="ExternalInput").ap()
    for name, shape, dt in SCRATCH:
        kind = "ExternalOutput" if name in dbg else "Internal"
        k.d[name] = nc.dram_tensor(name, shape, dt, kind=kind).ap()
    k.d["xw"] = nc.dram_tensor("xw", [D, T], F32, kind="ExternalOutput").ap()
    with ExitStack() as st:
        k.pool = SemPool(nc, st)
        k.cols = st.enter_context(nc.sbuf_tensor("sb_cols", [128, NCOLS], F32))
        k.modsb = st.enter_context(nc.sbuf_tensor("sb_mod", [128, 96, 2], F32))
        k.c32 = st.enter_context(nc.sbuf_tensor("sb_c32", [128, K_SEL], F32))
        k.epsc = st.enter_context(nc.sbuf_tensor("sb_epsc", [128, 1], F32))
        k.onec = st.enter_context(nc.sbuf_tensor("sb_onec", [128, 1], F32))
        with Phase(nc, k.pool, "init") as ph:
            ph.ld(k.c32[:], k.d["c32"][:, 0:K_SEL], "c32", ["c32"])
            ph.memset("vector", k.epsc[:], EPS, ["epsc"])
            ph.memset("vector", k.onec[:], 1.0, ["onec"])
        for l in layers:
            last = l == DEPTH - 1
            xsrc = k.d["xT"] if l == 0 else k.d["xw"]
            tiles = TILES[1:] if last else TILES
            supers = ([] if last else [(0, 256)]) + [(256 + 1024 * i, 1024) for i in range(4)]

            def on(p):
                return phases is None or p in phases
            if on("mod"):
                phase_mod(k, l)
            if on("norm1"):
                phase_norm1(k, l, xsrc)
            if on("proj"):
                phase_proj(k, l)
            if on("att"):
                phase_att(k, l)
            if on("lru"):
                phase_lru(k, l)
            if on("four"):
                phase_four1(k, l)
                phase_four2(k, l)
            if on("ssd"):
                phase_ssd_a(k, l)
                phase_ssd_b(k, l)
            if on("out"):
                phase_out(k, l, xsrc, tiles)
            if on("moe"):
                phase_moe(k, l, supers)
    return nc


def _col(vec):
    vec = np.asarray(vec, np.float32)
    return np.ascontiguousarray(vec.reshape(-1, 128).T)


def host_constants():
    c = {}
    c32 = np.zeros((128, NK), np.float32)
    c32[:, K_ONES:K_ONES + 128] = 1.0
    rm = np.zeros((128, 128), np.float32)
    for dd in range(64):
        rm[dd + 64, dd] = -1.0
        rm[dd, dd + 64] = 1.0
    c32[:, K_RM:K_RM + 128] = rm
    c32[:, K_ID:K_ID + 128] = np.eye(128, dtype=np.float32)
    for j in range(16):
        row = j if j < 8 else 32 + (j - 8)
        c32[row, K_SEL + j * 128:K_SEL + (j + 1) * 128] = 1.0
    c["c32"] = c32
    s = np.arange(128)[:, None]
    t = np.arange(512)[None, :]
    masks = np.zeros((8, 128, 512), np.float32)
    for kk in range(4):
        masks[kk] = np.where(kk * 128 + s <= t, 0.0, -1e30)
        masks[4 + kk] = np.where(kk * 128 + s >= t, 0.0, -1e30)
    c["masks"] = masks
    rows = SEQ // 64
    row = np.repeat(np.arange(rows), 64).astype(np.float32)
    colp = np.tile(np.arange(64), rows).astype(np.float32)
    inv = (10000.0 ** (-np.arange(32, dtype=np.float32) / 32)).astype(np.float32)
    ang = np.concatenate([row[:, None] * inv, colp[:, None] * inv], axis=-1)
    ang = np.concatenate([ang, ang], axis=-1).astype(np.float32)
    rope = np.zeros((2, 128, T), np.float32)
    rope[0, :, :NCTX] = 1.0
    rope[0, :, NCTX:] = np.cos(ang).T
    rope[1, :, NCTX:] = np.sin(ang).T
    c["rope"] = rope
    bf = ml_dtypes.bfloat16
    a = 2 * np.pi * np.outer(np.arange(128), np.arange(128)) / 128.0
    c["cs128"] = (np.concatenate([np.cos(a), np.sin(a)], axis=1) * 128 ** -0.5).astype(np.float32).astype(bf)

    def dft(n):
        idx = (np.outer(np.arange(n, dtype=np.int64), np.arange(n, dtype=np.int64)) % n).astype(np.float64)
        ang_ = 2 * np.pi * idx / n
        out = np.empty((2, n, n), bf)
        out[0] = (np.cos(ang_) * n ** -0.5).astype(np.float32).astype(bf)
        out[1] = (-np.sin(ang_) * n ** -0.5).astype(np.float32).astype(bf)
        return out
    c["dftL"] = dft(SEQ)
    c["dftc"] = dft(NCTX)
    return c


def host_shared(inp):
    sh = {}
    cols = np.zeros((DEPTH, 128, NCOLS), np.float32)
    f = lambda name: np.asarray(inp[name], np.float32)
    for l in range(DEPTH):
        cols[l, :, C_BMOD:C_BMOD + 96] = _col(f("b_mod")[l])
        cols[l, :, C_N1:C_N1 + 16] = _col(f("norm1")[l])
        cols[l, :, C_N2:C_N2 + 16] = _col(f("norm2")[l])
        cols[l, :, C_QN] = f("q_norm")[l]
        cols[l, :, C_KN] = f("k_norm")[l]
        lcw = f("lru_conv_w")[l]
        for n in range(4):
            for j in range(4):
                cols[l, :, C_LCW + 4 * n + j] = lcw[j, n * 128:(n + 1) * 128]
        cols[l, :, C_LCB:C_LCB + 4] = _col(f("lru_conv_b")[l])
        for dr in range(2):
            cols[l, :, C_LBA + dr * 4:C_LBA + dr * 4 + 4] = _col(f("lru_ba")[l, dr])
            cols[l, :, C_LBX + dr * 4:C_LBX + dr * 4 + 4] = _col(f("lru_bx")[l, dr])
            cols[l, :, C_LLAM + dr * 4:C_LLAM + dr * 4 + 4] = _col(f("lru_lam")[l, dr])
        scw = f("ssd_conv_w")[l]
        for n in range(8):
            for j in range(4):
                cols[l, :, C_SCW + 4 * n + j] = scw[j, n * 128:(n + 1) * 128]
        cols[l, :, C_SCB:C_SCB + 8] = _col(f("ssd_conv_b")[l])
        cols[l, :, C_SD:C_SD + 4] = _col(np.repeat(f("ssd_d")[l], 64))
        cols[l, :, C_SN:C_SN + 4] = _col(f("ssd_norm")[l])
        for dr in range(2):
            cols[l, dr * 32:dr * 32 + 8, C_ALOG] = f("ssd_a_log")[l, dr]
            cols[l, dr * 32:dr * 32 + 8, C_DTB] = f("ssd_dt_bias")[l, dr]
        bgu = f("exp_b_gu")[l]
        for e in range(NE):
            cols[l, :, C_BG + e * 6:C_BG + e * 6 + 6] = _col(bgu[e, 0::2])
            cols[l, :, C_BU + e * 6:C_BU + e * 6 + 6] = _col(bgu[e, 1::2])
    sh["cols"] = cols
    sh["rb"] = np.ascontiguousarray(np.broadcast_to(f("router_b")[:, None, :], (DEPTH, 128, NE)))
    for name in ("w_mod", "w_in", "w_out", "lru_wa", "lru_wx", "router_w", "exp_w_gu", "exp_w_dn", "exp_b_dn"):
        sh[name] = np.ascontiguousarray(f(name))
    return sh


def host_core(inp, b):
    x = np.asarray(inp["x"][b], np.float32)
    ctx = np.asarray(inp["ctx"][b], np.float32)
    xT = np.ascontiguousarray(np.concatenate([ctx, x], axis=0).T)
    cvec = np.stack([_col(np.asarray(inp["c"][b], np.float32)), _col(np.asarray(inp["c_ctx"], np.float32))], axis=-1)
    return {"xT": xT, "cvec": np.ascontiguousarray(cvec)}


def kernel(**inputs):
    B = inputs["x"].shape[0]
    nc = build_program()
    shared = host_shared(inputs)
    shared.update(host_constants())
    in_maps = []
    for b in range(B):
        m = dict(shared)
        m.update(host_core(inputs, b))
        in_maps.append(m)
    res = run_bass_kernel_spmd(nc, in_maps, core_ids=list(range(B)))
    out = np.empty((B, SEQ, D), np.float32)
    for b in range(B):
        out[b] = np.asarray(res.results[b]["xw"])[:, NCTX:].T
    return out
```

```python
import math
from contextlib import ExitStack

import numpy as np
import ml_dtypes
import concourse.bass as bass
import concourse.mybir as mybir
from concourse.bass_utils import run_bass_kernel_spmd

F32 = mybir.dt.float32
BF16 = mybir.dt.bfloat16
AF = mybir.ActivationFunctionType
ALU = mybir.AluOpType
AX = mybir.AxisListType

D = 2048
DEPTH = 4
T = 4352
NCTX = 256
SEQ = 4096
D_IN = 4112
NE = 32
EFF = 768
EPS = 1e-6
TILES = [(0, 256)] + [(256 + 512 * i, 512) for i in range(8)]
NCH = T // 128

C_BMOD, C_N1, C_N2, C_QN, C_KN = 0, 96, 112, 128, 129
C_LCW, C_LCB, C_LBA, C_LBX, C_LLAM = 130, 146, 150, 158, 166
C_SCW, C_SCB, C_SD, C_SN, C_ALOG, C_DTB = 174, 206, 214, 218, 222, 223
C_BG, C_BU, NCOLS = 224, 416, 608
K_ONES, K_RM, K_ID, K_SEL, NK = 0, 128, 256, 384, 384 + 16 * 128

ENGINES = ("tensor", "vector", "scalar", "gpsimd", "sync")


class _Op:
    __slots__ = ("eng", "fn", "reads", "writes", "dma", "waits", "sig", "mm")

    def __init__(self, eng, fn, reads, writes, dma, mm):
        self.eng, self.fn, self.reads, self.writes, self.dma, self.mm = eng, fn, reads, writes, dma, mm
        self.waits = []
        self.sig = None


class SemPool:
    def __init__(self, nc, stack, n_dma=88):
        self.eng_sem = {e: stack.enter_context(nc.semaphore("pe_" + e)) for e in ENGINES}
        self.eng_val = {e: 0 for e in ENGINES}
        self.dma_sems = [stack.enter_context(nc.semaphore("pd_%d" % i)) for i in range(n_dma)]
        self.dma_val = [0] * n_dma


class Phase:
    def __init__(self, nc, pool, name):
        self.nc, self.pool, self.name = nc, pool, name
        self.ops = []
        self.dma_key2slot = {}
        self.st = ExitStack()

    def __enter__(self):
        self.st.__enter__()
        return self

    def __exit__(self, *a):
        if a[0] is None:
            self.emit()
        return self.st.__exit__(*a)

    def sb(self, name, shape, dt=F32):
        return self.st.enter_context(self.nc.sbuf_tensor("%s_%s" % (self.name, name), list(shape), dt))

    def ps(self, name, shape, dt=F32):
        return self.st.enter_context(self.nc.psum_tensor("%s_%s" % (self.name, name), list(shape), dt))

    def op(self, eng, fn, reads=(), writes=(), mm=False):
        self.ops.append(_Op(eng, fn, tuple(reads), tuple(writes), None, mm))

    def dma(self, eng, fn, key, reads=(), writes=()):
        self.ops.append(_Op(eng, fn, tuple(reads), tuple(writes), key, False))

    def mm(self, out, lhsT, rhs, start, stop, r, w):
        self.op("tensor", lambda e: e.matmul(out, lhsT=lhsT, rhs=rhs, start=start, stop=stop), r, w, mm=True)

    def tr(self, out, in_, ident, r, w):
        self.op("tensor", lambda e: e.transpose(out, in_, ident), r, w, mm=True)

    def act(self, out, in_, func, r, w, bias=None, scale=None):
        kw = {}
        if bias is not None:
            kw["bias"] = bias
        if scale is not None:
            kw["scale"] = scale
        self.op("scalar", lambda e: e.activation(out=out, in_=in_, func=func, **kw), r, w)

    def tt(self, eng, out, in0, in1, op, r, w):
        self.op(eng, lambda e: e.tensor_tensor(out=out, in0=in0, in1=in1, op=op), r, w)

    def ts(self, eng, out, in0, s1, s2, op0, op1, r, w):
        if op1 is None:
            self.op(eng, lambda e: e.tensor_scalar(out=out, in0=in0, scalar1=s1, scalar2=None, op0=op0), r, w)
        else:
            self.op(eng, lambda e: e.tensor_scalar(out=out, in0=in0, scalar1=s1, scalar2=s2, op0=op0, op1=op1), r, w)

    def stt(self, out, in0, scalar, in1, op0, op1, r, w):
        self.op("vector", lambda e: e.scalar_tensor_tensor(out=out, in0=in0, scalar=scalar, in1=in1, op0=op0, op1=op1), r, w)

    def cp(self, eng, out, in_, r, w):
        if eng == "scalar":
            self.op(eng, lambda e: e.copy(out=out, in_=in_), r, w)
        else:
            self.op(eng, lambda e: e.tensor_copy(out=out, in_=in_), r, w)

    def recip(self, out, in_, r, w):
        self.op("vector", lambda e: e.reciprocal(out=out, in_=in_), r, w)

    def scan(self, out, d0, d1, init, r, w):
        self.op("vector", lambda e: e.tensor_tensor_scan(out=out, data0=d0, data1=d1, initial=init, op0=ALU.mult, op1=ALU.add), r, w)

    def memset(self, eng, ap, val, w):
        self.op(eng, lambda e: e.memset(ap, val), (), w)

    def ld(self, out, in_, key, w, r=(), eng="sync"):
        self.dma(eng, lambda e: e.dma_start(out=out, in_=in_), key, reads=r, writes=w)

    def store(self, out, in_, key, r, w=(), eng="sync"):
        self.dma(eng, lambda e: e.dma_start(out=out, in_=in_), key, reads=r, writes=w)

    def emit(self):
        pool, ops = self.pool, self.ops
        last_w, readers, deps_of, needed = {}, {}, [], set()
        for i, o in enumerate(ops):
            deps = set()
            for r in o.reads:
                w = last_w.get(r)
                if w is not None:
                    deps.add(w)
            for w_ in o.writes:
                w = last_w.get(w_)
                if w is not None:
                    deps.add(w)
                deps.update(readers.get(w_, ()))
            deps.discard(i)
            fd = []
            for d in deps:
                od = ops[d]
                if od.mm and o.mm:
                    continue
                if od.dma is not None and o.dma is not None and od.dma == o.dma and od.eng == o.eng \
                        and not (set(od.writes) & set(o.reads)) and not (set(od.reads) & set(o.writes)):
                    continue
                fd.append(d)
                needed.add(d)
            deps_of.append(fd)
            for r in o.reads:
                readers.setdefault(r, []).append(i)
            for w_ in o.writes:
                last_w[w_] = i
                readers[w_] = []
        for i, o in enumerate(ops):
            if o.dma is not None:
                slot = self.dma_key2slot.get(o.dma)
                if slot is None:
                    slot = len(self.dma_key2slot)
                    assert slot < len(pool.dma_sems), "out of dma semaphores in " + self.name
                    self.dma_key2slot[o.dma] = slot
                pool.dma_val[slot] += 16
                o.sig = (pool.dma_sems[slot], 16, pool.dma_val[slot], ("d", slot))
            elif i in needed:
                pool.eng_val[o.eng] += 1
                o.sig = (pool.eng_sem[o.eng], 1, pool.eng_val[o.eng], ("e", o.eng))
        waited = {e: {} for e in ENGINES}
        cur = {}
        for i, o in enumerate(ops):
            wl = {}
            for d in deps_of[i]:
                s = ops[d].sig
                if s is None:
                    continue
                sem, _, val, sid = s
                if sid[0] == "d":
                    val = max(val, cur.get(sid, 0))
                if waited[o.eng].get(sid, 0) >= val:
                    continue
                if wl.get(sid, (None, 0))[1] < val:
                    wl[sid] = (sem, val)
            for sid, (sem, val) in wl.items():
                waited[o.eng][sid] = val
                o.waits.append((sem, val))
            if o.dma is not None:
                cur[o.sig[3]] = o.sig[2]
        tail = {e: [] for e in ENGINES}
        seen = set()
        for o in reversed(ops):
            if o.dma is not None:
                sem, _, val, sid = o.sig
                if sid in seen:
                    continue
                seen.add(sid)
                if waited[o.eng].get(sid, 0) < val:
                    tail[o.eng].append((sem, val))
        by_eng = {e: [o for o in ops if o.eng == e] for e in ENGINES}
        with self.nc.Block() as block:
            for e in ENGINES:
                lst, tl = by_eng[e], tail[e]
                if not lst and not tl:
                    continue

                def body(eng, lst=lst, tl=tl):
                    for o in lst:
                        for sem, val in o.waits:
                            eng.wait_ge(sem, val)
                        ins = o.fn(eng)
                        if o.sig is not None:
                            ins.then_inc(o.sig[0], o.sig[1])
                    for sem, val in tl:
                        eng.wait_ge(sem, val)

                getattr(block, e)(body)
        self.ops = []


class Ring:
    def __init__(self, items):
        self.items, self.i = items, 0

    def next(self):
        it = self.items[self.i % len(self.items)]
        self.i += 1
        return it


class K:
    pass


def bc(ap, n):
    return ap.unsqueeze(1).broadcast_to([ap.shape[0], n, ap.shape[1]])


def phase_mod(k, l):
    with Phase(k.nc, k.pool, "mod%d" % l) as ph:
        cv = ph.sb("cv", [128, 16, 2])
        s = ph.sb("s", [128, 16, 2])
        ph.ld(k.cols[:], k.d["cols"][l], "cols", ["cols"])
        ph.ld(cv[:], k.d["cvec"], "cv", ["cv"])
        ph.act(s[:], cv[:], AF.Silu, ["cv"], ["s"])
        wt = [ph.sb("w%d" % i, [128, 16, 1024]) for i in range(2)]
        psm = ph.ps("psm", [128, 96, 2])
        wsrc = k.d["w_mod"][l].rearrange("(kc p) f -> p kc f", p=128)
        for fb in range(12):
            w, wn = wt[fb % 2], "w%d" % (fb % 2)
            ph.ld(w[:, 0:8, :], wsrc[:, 0:8, fb * 1024:(fb + 1) * 1024], wn, [wn])
            ph.ld(w[:, 8:16, :], wsrc[:, 8:16, fb * 1024:(fb + 1) * 1024], wn, [wn])
            for fc in range(8):
                j = fb * 8 + fc
                for kc in range(16):
                    ph.mm(psm[:, j, :], w[:, kc, fc * 128:(fc + 1) * 128], s[:, kc, :], kc == 0, kc == 15, [wn, "s"], ["psm"])
        m = k.modsb
        for v in range(2):
            ph.tt("vector", m[:, :, v], psm[:, :, v], k.cols[:, C_BMOD:C_BMOD + 96], ALU.add, ["psm", "cols"], ["modsb"])
        for v in range(2):
            ph.stt(m[:, 16:32, v], m[:, 16:32, v], 1.0, k.cols[:, C_N1:C_N1 + 16], ALU.add, ALU.mult, ["modsb", "cols"], ["modsb"])
            ph.stt(m[:, 64:80, v], m[:, 64:80, v], 1.0, k.cols[:, C_N2:C_N2 + 16], ALU.add, ALU.mult, ["modsb", "cols"], ["modsb"])


def norm_tile(ph, k, xt, xtn, W, v, a_off, b_off, out, outn, tmp, tmpn, rs, rsn, psn, psnn, out_f32=None):
    ph.act(tmp[:, :, :W], xt[:, :, :W], AF.Square, [xtn], [tmpn])
    for c in range(16):
        ph.mm(psn[:, :W], k.c32[:, K_ONES:K_ONES + 128], tmp[:, c, :W], c == 0, c == 15, [tmpn, "c32"], [psnn])
    ph.act(rs[:, :W], psn[:, :W], AF.Sqrt, [psnn], [rsn], bias=k.epsc[:, 0:1], scale=1.0 / D)
    ph.recip(rs[:, :W], rs[:, :W], [rsn], [rsn])
    ph.tt("vector", tmp[:, :, :W], xt[:, :, :W], bc(rs[:, :W], 16), ALU.mult, [xtn, rsn], [tmpn])
    for c in range(16):
        ph.act(tmp[:, c, :W], tmp[:, c, :W], AF.Identity, [tmpn, "modsb"], [tmpn],
               bias=k.modsb[:, b_off + c, v:v + 1], scale=k.modsb[:, a_off + c, v:v + 1])
    ph.cp("gpsimd", out[:, :, :W], tmp[:, :, :W], [tmpn], [outn])


def phase_norm1(k, l, xsrc):
    with Phase(k.nc, k.pool, "n1_%d" % l) as ph:
        xts = [ph.sb("xt%d" % i, [128, 16, 512]) for i in range(2)]
        tmps = [ph.sb("tmp%d" % i, [128, 16, 512]) for i in range(2)]
        hbs = [ph.sb("hb%d" % i, [128, 16, 512], BF16) for i in range(2)]
        rss = [ph.sb("rs%d" % i, [128, 512]) for i in range(2)]
        psn = [ph.ps("psn%d" % i, [128, 512]) for i in range(2)]
        xv = xsrc.rearrange("(c p) t -> p c t", p=128)
        hv = k.d["hT"].rearrange("(c p) t -> p c t", p=128)
        tiles = TILES
        ph.ld(xts[0][:, :, :tiles[0][1]], xv[:, :, 0:tiles[0][1]], "xt0", ["xt0"])
        for i, (t0, W) in enumerate(tiles):
            b = i % 2
            if i + 1 < len(tiles):
                t1, W1 = tiles[i + 1]
                ph.ld(xts[1 - b][:, :, :W1], xv[:, :, t1:t1 + W1], "xt%d" % (1 - b), ["xt%d" % (1 - b)])
            norm_tile(ph, k, xts[b], "xt%d" % b, W, 1 if i == 0 else 0, 16, 0, hbs[b], "hb%d" % b,
                      tmps[b], "tmp%d" % b, rss[b], "rs%d" % b, psn[b], "psn%d" % b)
            ph.store(hv[:, :, t0:t0 + W], hbs[b][:, :, :W], "hb%d" % b, ["hb%d" % b])


def phase_proj(k, l):
    d = k.d
    with Phase(k.nc, k.pool, "pj%d" % l) as ph:
        wbs = [ph.sb("wb%d" % i, [128, 16, 1024], BF16) for i in range(2)]
        hts = [ph.sb("ht%d" % i, [128, 16, 512], BF16) for i in range(2)]
        css = [ph.sb("cs%d" % i, [128, 2, 512]) for i in range(2)]
        psA = Ring([(ph.ps("psA%d" % i, [128, 512]), "psA%d" % i) for i in range(3)])
        psN = (ph.ps("psN", [128, 512]), "psN")
        psR = (ph.ps("psR", [128, 512]), "psR")
        psV = (ph.ps("psV", [128, 256]), "psV")
        s32 = Ring([(ph.sb("s32_%d" % i, [128, 512]), "s32_%d" % i) for i in range(3)])
        s16 = Ring([(ph.sb("s16_%d" % i, [128, 512], BF16), "s16_%d" % i) for i in range(3)])
        sv = Ring([(ph.sb("sv_%d" % i, [128, 256], BF16), "sv_%d" % i) for i in range(2)])
        tq = Ring([tuple((ph.sb("tq%d_%d" % (i, j), [128, 512]), "tq%d_%d" % (i, j)) for j in range(5)) for i in range(2)])
        tg = Ring([tuple((ph.sb("tg%d_%d" % (i, j), [128, 512]), "tg%d_%d" % (i, j)) for j in range(2)) for i in range(2)])
        wsrc = d["w_in"][l].rearrange("(kc p) f -> p kc f", p=128)
        hv = d["hT"].rearrange("(c p) t -> p c t", p=128)
        groups = [(0, 1024), (1024, 1024), (2048, 1024), (3072, 1024), (4096, 16)]
        items = [(cg, ti) for cg in range(5) for ti in range(len(TILES))]
        ones32 = k.c32[:, K_ONES:K_ONES + 128]
        rm32 = k.c32[:, K_RM:K_RM + 128]
        cnt = [0]

        def load_w(cg):
            c0, n = groups[cg]
            wb, wn = wbs[cg % 2], "wb%d" % (cg % 2)
            for h in range(2):
                ph.ld(wb[:, 8 * h:8 * h + 8, :n], wsrc[:, 8 * h:8 * h + 8, c0:c0 + n], wn, [wn], eng="gpsimd")

        def load_item(ii):
            cg, ti = items[ii]
            t0, W = TILES[ti]
            b = ii % 2
            ph.ld(hts[b][:, :, :W], hv[:, :, t0:t0 + W], "ht%d" % b, ["ht%d" % b])
            if cg == 0:
                ph.ld(css[b][:, :, :W], d["rope"].rearrange("a p t -> p a t")[:, :, t0:t0 + W], "cs%d" % b, ["cs%d" % b])

        load_w(0)
        load_item(0)
        for ii, (cg, ti) in enumerate(items):
            t0, W = TILES[ti]
            b = ii % 2
            ht, htn = hts[b], "ht%d" % b
            wb, wn = wbs[cg % 2], "wb%d" % (cg % 2)
            if ti == 0 and cg + 1 < 5:
                load_w(cg + 1)
            if ii + 1 < len(items):
                load_item(ii + 1)
            nchunks = 1 if cg == 4 else 8
            for n in range(nchunks):
                if cg == 0 and n >= 6:
                    continue
                M = 16 if cg == 4 else 128
                ps, psn = psA.next()
                for kc in range(16):
                    ph.mm(ps[:M, :W], wb[:, kc, n * 128:n * 128 + M], ht[:, kc, :W], kc == 0, kc == 15, [wn, htn], [psn])
                cnt[0] += 1
                if cg == 0:
                    gcol = k.cols[:, C_QN:C_QN + 1] if n < 4 else k.cols[:, C_KN:C_KN + 1]
                    (xq, xqn), (sq, sqn), (rq, rqn), (xn, xnn), (t1, t1n) = tq.next()
                    cs, csn = css[b], "cs%d" % b
                    ph.cp("scalar", xq[:, :W], ps[:, :W], [psn], [xqn])
                    ph.act(sq[:, :W], ps[:, :W], AF.Square, [psn], [sqn])
                    ph.mm(psN[0][:, :W], ones32, sq[:, :W], True, True, [sqn, "c32"], [psN[1]])
                    ph.act(rq[:, :W], psN[0][:, :W], AF.Sqrt, [psN[1]], [rqn], bias=k.epsc[:, 0:1], scale=1.0 / 128)
                    ph.recip(rq[:, :W], rq[:, :W], [rqn], [rqn])
                    ph.stt(xn[:, :W], xq[:, :W], gcol, rq[:, :W], ALU.mult, ALU.mult, [xqn, rqn, "cols"], [xnn])
                    ph.mm(psR[0][:, :W], rm32, xn[:, :W], True, True, [xnn, "c32"], [psR[1]])
                    ph.tt("gpsimd", t1[:, :W], xn[:, :W], cs[:, 0, :W], ALU.mult, [xnn, csn], [t1n])
                    ph.tt("vector", sq[:, :W], psR[0][:, :W], cs[:, 1, :W], ALU.mult, [psR[1], csn], [sqn])
                    st, stn = s16.next()
                    ph.tt("vector", st[:, :W], t1[:, :W], sq[:, :W], ALU.add, [t1n, sqn], [stn])
                    dst = d["qT"][n * 128:(n + 1) * 128, t0:t0 + W] if n < 4 else d["kT"][(n - 4) * 128:(n - 3) * 128, t0:t0 + W]
                    ph.store(dst, st[:, :W], stn, [stn])
                elif cg == 1 and n >= 4:
                    (ta, tan), (tb, tbn) = tg.next()
                    ph.act(ta[:, :W], ps[:, :W], AF.Square, [psn], [tan])
                    ph.ts("vector", ta[:, :W], ta[:, :W], 0.044715, 1.0, ALU.mult, ALU.add, [tan], [tan])
                    ph.tt("vector", tb[:, :W], ta[:, :W], ps[:, :W], ALU.mult, [tan, psn], [tbn])
                    ph.act(tb[:, :W], tb[:, :W], AF.Sigmoid, [tbn], [tbn], scale=2.0 * math.sqrt(2.0 / math.pi))
                    st, stn = s32.next()
                    ph.tt("vector", st[:, :W], tb[:, :W], ps[:, :W], ALU.mult, [tbn, psn], [stn])
                    ph.store(d["lgT"][(n - 4) * 128:(n - 3) * 128, t0:t0 + W], st[:, :W], stn, [stn])
                elif cg == 2 and n < 4:
                    st, stn = s16.next()
                    ph.cp("vector" if cnt[0] % 2 else "scalar", st[:, :W], ps[:, :W], [psn], [stn])
                    ph.store(d["fT"][n * 128:(n + 1) * 128, t0:t0 + W], st[:, :W], stn, [stn])
                elif cg == 2:
                    st, stn = s32.next()
                    ph.act(st[:, :W], ps[:, :W], AF.Silu, [psn], [stn])
                    ph.store(d["zT"][(n - 4) * 128:(n - 3) * 128, t0:t0 + W], st[:, :W], stn, [stn])
                else:
                    st, stn = s32.next()
                    ph.cp("vector" if cnt[0] % 2 else "scalar", st[:M, :W], ps[:M, :W], [psn], [stn])
                    if cg == 1:
                        dst = d["lxT"][n * 128:(n + 1) * 128, t0:t0 + W]
                    elif cg == 3:
                        dst = d["xbcT"][n * 128:(n + 1) * 128, t0:t0 + W]
                    else:
                        dst = d["dtT"][:, t0:t0 + W]
                    ph.store(dst, st[:M, :W], stn, [stn])
            if cg == 0:
                for tc in range(W // 128):
                    for kc in range(16):
                        ph.mm(psV[0][:, :], ht[:, kc, tc * 128:(tc + 1) * 128], wb[:, kc, 768:1024], kc == 0, kc == 15, [wn, htn], [psV[1]])
                    st, stn = sv.next()
                    ph.cp("scalar", st[:, :], psV[0][:, :], [psV[1]], [stn])
                    ph.store(d["v"][t0 + tc * 128:t0 + (tc + 1) * 128, :], st[:, :], stn, [stn])


def phase_att(k, l):
    d = k.d
    scale = 128 ** -0.5
    with Phase(k.nc, k.pool, "att%d" % l) as ph:
        kT = ph.sb("kT", [128, 2, T], BF16)
        vs = ph.sb("vs", [128, NCH, 256], BF16)
        onesb = ph.sb("onesb", [128, 128], BF16)
        qs = [ph.sb("q%d" % i, [128, 4, 512], BF16) for i in range(2)]
        pts = Ring([(ph.sb("pt%d" % i, [128, 512], BF16), "pt%d" % i) for i in range(4)])
        psS = Ring([(ph.ps("psS%d" % i, [128, 512]), "psS%d" % i) for i in range(3)])
        psO = Ring([((ph.ps("psO%d" % i, [128, 512]), "psO%d" % i), (ph.ps("psL%d" % i, [128, 512]), "psL%d" % i)) for i in range(2)])
        rsb = Ring([(ph.sb("rsb%d" % i, [128, 512]), "rsb%d" % i) for i in range(2)])
        ost = Ring([(ph.sb("ost%d" % i, [128, 512], BF16), "ost%d" % i) for i in range(3)])
        ph.cp("vector", onesb[:], k.c32[:, K_ONES:K_ONES + 128], ["c32"], ["onesb"])
        kv = d["kT"].rearrange("(h p) t -> p h t", p=128)
        ph.ld(kT[:, 0, :], kv[:, 0, :], "kT", ["kT"])
        ph.ld(kT[:, 1, :], kv[:, 1, :], "kT", ["kT"])
        vv = d["v"].rearrange("(c p) e -> p c e", p=128)
        ph.ld(vs[:, 0:17, :], vv[:, 0:17, :], "vs", ["vs"])
        ph.ld(vs[:, 17:34, :], vv[:, 17:34, :], "vs", ["vs"])
        qv = d["qT"].rearrange("(h p) t -> p h t", p=128)
        ph.ld(qs[0][:, :, :256], qv[:, :, 0:256], "q0", ["q0"])
        for ti, (t0, W) in enumerate(TILES):
            b = ti % 2
            if ti + 1 < len(TILES):
                t1, W1 = TILES[ti + 1]
                ph.ld(qs[1 - b][:, :, :W1], qv[:, :, t1:t1 + W1], "q%d" % (1 - b), ["q%d" % (1 - b)])
            nsc = 2 if ti == 0 else NCH
            for h in range(4):
                kvh = h // 2
                (po, pon), (pl, pln) = psO.next()
                pend = {}

                def stA(sc, h=h, kvh=kvh, W=W, b=b):
                    ps, psn = psS.next()
                    ph.mm(ps[:, :W], kT[:, kvh, sc * 128:(sc + 1) * 128], qs[b][:, h, :W], True, True, ["kT", "q%d" % b], [psn])
                    pt, ptn = pts.next()
                    ph.act(pt[:, :W], ps[:, :W], AF.Exp, [psn], [ptn], scale=scale)
                    pend[sc] = (pt, ptn)

                def stB(sc, kvh=kvh, W=W, nsc=nsc, po=po, pon=pon, pl=pl, pln=pln):
                    pt, ptn = pend.pop(sc)
                    ph.mm(po[:, :W], vs[:, sc, kvh * 128:(kvh + 1) * 128], pt[:, :W], sc == 0, sc == nsc - 1, ["vs", ptn], [pon])
                    ph.mm(pl[:, :W], onesb[:], pt[:, :W], sc == 0, sc == nsc - 1, ["onesb", ptn], [pln])

                stA(0)
                if nsc > 1:
                    stA(1)
                for sc in range(nsc):
                    if sc + 2 < nsc:
                        stA(sc + 2)
                    stB(sc)
                rs, rsn = rsb.next()
                ph.recip(rs[:, :W], pl[:, :W], [pln], [rsn])
                st, stn = ost.next()
                ph.tt("vector", st[:, :W], po[:, :W], rs[:, :W], ALU.mult, [pon, rsn], [stn])
                ph.store(d["featT"][h * 128:(h + 1) * 128, t0:t0 + W], st[:, :W], stn, [stn])


def conv_seg(ph, k, u, un, x, xn, wcol0, bcol):
    ph.act(u[:, :], x[:, :], AF.Identity, [xn, "cols"], [un], bias=k.cols[:, bcol:bcol + 1], scale=k.cols[:, wcol0 + 2:wcol0 + 3])
    for j in (0, 1, 3):
        off = j - 2
        for (s0, s1) in ((0, NCTX), (NCTX, T)):
            lo, hi = max(s0, s0 - off), min(s1, s1 - off)
            ph.stt(u[:, lo:hi], x[:, lo + off:hi + off], k.cols[:, wcol0 + j:wcol0 + j + 1], u[:, lo:hi], ALU.mult, ALU.add, [xn, un, "cols"], [un])


def phase_lru(k, l):
    d = k.d
    with Phase(k.nc, k.pool, "lru%d" % l) as ph:
        x = ph.sb("x", [128, T])
        u = ph.sb("u", [128, T])
        lg = ph.sb("lg", [128, T])
        ra = ph.sb("ra", [128, T])
        ib = ph.sb("ib", [128, T])
        hf = ph.sb("hf", [128, T])
        hb = ph.sb("hb", [128, T])
        ob = ph.sb("ob", [128, T], BF16)
        wa = ph.sb("wa", [128, 2, 4, 128])
        wx = ph.sb("wx", [128, 2, 4, 128])
        sp = ph.sb("sp", [128, 8])
        psg = Ring([(ph.ps("psg%d" % i, [128, 512]), "psg%d" % i) for i in range(4)])
        ph.ld(wa[:], d["lru_wa"][l].rearrange("d n c e -> c d n e"), "wa", ["wa"])
        ph.ld(wx[:], d["lru_wx"][l].rearrange("d n c e -> c d n e"), "wx", ["wx"])
        ph.act(sp[:], k.cols[:, C_LLAM:C_LLAM + 8], AF.Exp, ["cols"], ["sp"], scale=-1.0)
        ph.act(sp[:], sp[:], AF.Ln, ["sp"], ["sp"], bias=k.onec[:, 0:1])
        ph.ts("vector", sp[:], sp[:], -8.0, None, ALU.mult, None, ["sp"], ["sp"])
        for n in range(4):
            ph.ld(x[:], d["lxT"][n * 128:(n + 1) * 128, :], "x", ["x"])
            ph.ld(lg[:], d["lgT"][n * 128:(n + 1) * 128, :], "lg", ["lg"])
            conv_seg(ph, k, u, "u", x, "x", C_LCW + 4 * n, C_LCB + n)
            for dr in range(2):
                for (t0, W) in TILES:
                    p1, p1n = psg.next()
                    ph.mm(p1[:, :W], wa[:, dr, n, :], u[:, t0:t0 + W], True, True, ["wa", "u"], [p1n])
                    ph.act(ra[:, t0:t0 + W], p1[:, :W], AF.Sigmoid, [p1n, "cols"], ["ra"], bias=k.cols[:, C_LBA + dr * 4 + n:C_LBA + dr * 4 + n + 1])
                    p2, p2n = psg.next()
                    ph.mm(p2[:, :W], wx[:, dr, n, :], u[:, t0:t0 + W], True, True, ["wx", "u"], [p2n])
                    ph.act(ib[:, t0:t0 + W], p2[:, :W], AF.Sigmoid, [p2n, "cols"], ["ib"], bias=k.cols[:, C_LBX + dr * 4 + n:C_LBX + dr * 4 + n + 1])
                ph.act(ra[:], ra[:], AF.Exp, ["ra", "sp"], ["ra"], scale=sp[:, dr * 4 + n:dr * 4 + n + 1])
                ph.act(x[:], ra[:], AF.Square, ["ra"], ["x"])
                ph.ts("vector", x[:], x[:], -1.0, 1.0, ALU.mult, ALU.add, ["x"], ["x"])
                ph.act(x[:], x[:], AF.Sqrt, ["x"], ["x"])
                ph.tt("gpsimd", ib[:], ib[:], u[:], ALU.mult, ["ib", "u"], ["ib"])
                ph.tt("vector", ib[:], ib[:], x[:], ALU.mult, ["ib", "x"], ["ib"])
                if dr == 0:
                    ph.scan(hf[:], ra[:], ib[:], 0.0, ["ra", "ib"], ["hf"])
                else:
                    ph.scan(hb[:, 0:NCTX][:, ::-1], ra[:, 0:NCTX][:, ::-1], ib[:, 0:NCTX][:, ::-1], 0.0, ["ra", "ib"], ["hb"])
                    ph.scan(hb[:, NCTX:T][:, ::-1], ra[:, NCTX:T][:, ::-1], ib[:, NCTX:T][:, ::-1], hb[:, 0:1], ["ra", "ib", "hb"], ["hb"])
            ph.tt("gpsimd", hf[:], hf[:], hb[:], ALU.add, ["hf", "hb"], ["hf"])
            ph.tt("vector", ob[:], hf[:], lg[:], ALU.mult, ["hf", "lg"], ["ob"])
            ph.store(d["featT"][512 + n * 128:512 + (n + 1) * 128, :], ob[:], "ob", ["ob"])


def phase_four1(k, l):
    d = k.d
    with Phase(k.nc, k.pool, "f1_%d" % l) as ph:
        fs = ph.sb("fs", [128, 4, T], BF16)
        cs = ph.sb("cs", [128, 256], BF16)
        pq = Ring([(ph.ps("pq%d" % i, [128, 4, 256]), "pq%d" % i) for i in range(3)])
        st = Ring([(ph.sb("st%d" % i, [128, 4, 256], BF16), "st%d" % i) for i in range(3)])
        ph.ld(cs[:], d["cs128"], "cs", ["cs"])
        fv = d["fT"].rearrange("(g p) t -> p g t", p=128)
        for g in range(4):
            ph.ld(fs[:, g, :], fv[:, g, :], "fs", ["fs"])
        pv = d["PQ"].rearrange("(c p) g m -> p c g m", p=128)
        for tc in range(NCH):
            p, pn = pq.next()
            for g in range(4):
                ph.mm(p[:, g, :], fs[:, g, tc * 128:(tc + 1) * 128], cs[:], True, True, ["fs", "cs"], [pn])
            s, sn = st.next()
            ph.cp("vector" if tc % 2 else "scalar", s[:], p[:], [pn], [sn])
            ph.store(pv[:, tc, :, :], s[:], sn, [sn])


def phase_four2(k, l):
    d = k.d
    with Phase(k.nc, k.pool, "f2_%d" % l) as ph:
        PQ = ph.sb("PQ", [128, NCH, 4, 256], BF16)
        dfc = ph.sb("dfc", [128, 2, 2, 256], BF16)
        units = Ring([(ph.sb("du%d" % i, [128, 32, 512], BF16), "du%d" % i) for i in range(3)])
        psy = [[(ph.ps("py%d_%d" % (s, g), [128, 512]), "py%d_%d" % (s, g)) for g in range(4)] for s in range(2)]
        st = Ring([(ph.sb("st%d" % i, [128, 512], BF16), "st%d" % i) for i in range(4)])
        pv = d["PQ"].rearrange("(c p) g m -> p c g m", p=128)
        ph.ld(PQ[:, 0:17], pv[:, 0:17], "PQ", ["PQ"])
        ph.ld(PQ[:, 17:34], pv[:, 17:34], "PQ", ["PQ"])
        ph.ld(dfc[:], d["dftc"].rearrange("m (c p) q -> p m c q", p=128), "dfc", ["dfc"])
        ul = [(kt, m) for kt in range(8) for m in range(2)]
        dv = [d["dftL"][m].rearrange("(c p) q -> p c q", p=128) for m in range(2)]
        cur = {}

        def load_u(i):
            kt, m = ul[i]
            u, un = units.next()
            ph.ld(u[:, 0:16, :], dv[m][:, 0:16, kt * 512:(kt + 1) * 512], un, [un])
            ph.ld(u[:, 16:32, :], dv[m][:, 16:32, kt * 512:(kt + 1) * 512], un, [un])
            cur[i] = (u, un)

        load_u(0)
        load_u(1)
        for g in range(4):
            p, pn = psy[1][g]
            for m in range(2):
                for c in range(2):
                    ph.mm(p[:, :256], PQ[:, c, g, m * 128:(m + 1) * 128], dfc[:, m, c, :], m == 0 and c == 0, m == 1 and c == 1, ["PQ", "dfc"], [pn])
            s, sn = st.next()
            ph.cp("vector", s[:, :256], p[:, :256], [pn], [sn])
            ph.store(d["featT"][1024 + g * 128:1024 + (g + 1) * 128, 0:256], s[:, :256], sn, [sn])
        for i, (kt, m) in enumerate(ul):
            if i + 2 < len(ul):
                load_u(i + 2)
            u, un = cur.pop(i)
            for g in range(4):
                p, pn = psy[kt % 2][g]
                for tc in range(32):
                    ph.mm(p[:, :], PQ[:, 2 + tc, g, m * 128:(m + 1) * 128], u[:, tc, :], m == 0 and tc == 0, m == 1 and tc == 31, ["PQ", un], [pn])
                if m == 1:
                    s, sn = st.next()
                    ph.cp("vector" if g % 2 else "scalar", s[:, :], p[:, :], [pn], [sn])
                    ph.store(d["featT"][1024 + g * 128:1024 + (g + 1) * 128, 256 + kt * 512:256 + (kt + 1) * 512], s[:, :], sn, [sn])


def phase_ssd_a(k, l):
    d = k.d
    with Phase(k.nc, k.pool, "sa%d" % l) as ph:
        x = ph.sb("x", [128, T])
        u = ph.sb("u", [128, T])
        ub = ph.sb("ub", [128, T], BF16)
        xtm = ph.sb("xtm", [128, NCH, 512], BF16)
        idb = ph.sb("idb", [128, 128], BF16)
        dt = ph.sb("dt", [64, T])
        y = ph.sb("y", [64, T])
        e = ph.sb("e", [64, T])
        P = ph.sb("P", [64, T])
        ones = ph.sb("ones", [64, T])
        acol = ph.sb("acol", [64, 1])
        bcs = ph.sb("bcs", [128, NCH, 40])
        pst = Ring([(ph.ps("pst%d" % i, [128, 4, 128], BF16), "pst%d" % i) for i in range(2)])
        psb = Ring([(ph.ps("psb%d" % i, [128, 8, 40]), "psb%d" % i) for i in range(2)])
        ph.cp("vector", idb[:], k.c32[:, K_ID:K_ID + 128], ["c32"], ["idb"])
        ph.memset("gpsimd", dt[:], 0.0, ["dt"])
        ph.memset("gpsimd", P[:], 0.0, ["P"])
        ph.memset("gpsimd", ones[:], 1.0, ["ones"])
        ph.ld(dt[0:8, :], d["dtT"][0:8, :], "dt", ["dt"], r=["dt"])
        ph.ld(dt[32:40, :], d["dtT"][8:16, :], "dt", ["dt"], r=["dt"])
        ph.act(y[0:40, :], dt[0:40, :], AF.Identity, ["dt", "cols"], ["y"], bias=k.cols[0:40, C_DTB:C_DTB + 1])
        ph.act(e[0:40, :], y[0:40, :], AF.Abs, ["y"], ["e"])
        ph.act(e[0:40, :], e[0:40, :], AF.Exp, ["e"], ["e"], scale=-1.0)
        ph.act(e[0:40, :], e[0:40, :], AF.Ln, ["e"], ["e"], bias=k.onec[0:40, 0:1])
        ph.ts("vector", y[0:40, :], y[0:40, :], 0.0, None, ALU.max, None, ["y"], ["y"])
        ph.tt("vector", dt[0:40, :], y[0:40, :], e[0:40, :], ALU.add, ["y", "e"], ["dt"])
        ph.act(e[0:40, :], dt[0:40, :], AF.Ln, ["dt"], ["e"])
        ph.act(acol[0:40, :], k.cols[0:40, C_ALOG:C_ALOG + 1], AF.Exp, ["cols"], ["acol"])
        ph.ts("vector", acol[0:40, :], acol[0:40, :], -1.0, None, ALU.mult, None, ["acol"], ["acol"])
        ph.ts("vector", y[0:40, :], dt[0:40, :], acol[0:40, 0:1], None, ALU.mult, None, ["dt", "acol"], ["y"])
        ph.scan(P[0:8, :], ones[0:8, :], y[0:8, :], 0.0, ["ones", "y", "P"], ["P"])
        ph.scan(P[32:40, NCTX:T], ones[32:40, NCTX:T], y[32:40, NCTX:T], 0.0, ["ones", "y", "P"], ["P"])
        ph.scan(P[32:40, 0:NCTX], ones[32:40, 0:NCTX], y[32:40, 0:NCTX], P[32:40, T - 1:T], ["ones", "y", "P"], ["P"])
        ph.tt("vector", P[32:40, :], y[32:40, :], P[32:40, :], ALU.subtract, ["y", "P"], ["P"])
        ph.store(d["Rrow"], P[0:40, :], "P", ["P"])
        ph.tt("vector", e[0:40, :], e[0:40, :], P[0:40, :], ALU.subtract, ["e", "P"], ["e"])
        for c8 in range(0, NCH, 8):
            nb = min(8, NCH - c8)
            p, pn = psb.next()
            for j in range(nb):
                tc = c8 + j
                ph.tr(p[:, j, :], e[0:40, tc * 128:(tc + 1) * 128], k.c32[0:40, K_ID:K_ID + 40], ["e", "c32"], [pn])
            ph.cp("vector", bcs[:, c8:c8 + nb, :], p[:, 0:nb, :], [pn], ["bcs"])
        ph.store(d["bcol"].rearrange("(c p) r -> p c r", p=128), bcs[:], "bcs", ["bcs"])
        for c in range(8):
            ph.ld(x[:], d["xbcT"][c * 128:(c + 1) * 128, :], "x", ["x"])
            conv_seg(ph, k, u, "u", x, "x", C_SCW + 4 * c, C_SCB + c)
            ph.act(u[:], u[:], AF.Silu, ["u"], ["u"])
            if c < 4:
                ph.store(d["xsT"][c * 128:(c + 1) * 128, :], u[:], "u", ["u"])
            ph.cp("gpsimd", ub[:], u[:], ["u"], ["ub"])
            if c < 4:
                for c4 in range(0, NCH, 4):
                    nb = min(4, NCH - c4)
                    p, pn = pst.next()
                    for j in range(nb):
                        tc = c4 + j
                        ph.tr(p[:, j, :], ub[:, tc * 128:(tc + 1) * 128], idb[:], ["ub", "idb"], [pn])
                    ph.cp("vector" if (c4 // 4) % 2 else "scalar", xtm[:, c4:c4 + nb, c * 128:(c + 1) * 128], p[:, 0:nb, :], [pn], ["xtm"])
            elif c < 6:
                ph.store(d["BT"][c - 4], ub[:], "ub", ["ub"])
            else:
                ph.store(d["CT"][c - 6], ub[:], "ub", ["ub"])
        ph.store(d["x_tm"].rearrange("(c p) f -> p c f", p=128), xtm[:], "xtm", ["xtm"])


def ssd_pairs(ti):
    t0, W = TILES[ti]
    prs = []
    if ti == 0:
        for dr in range(2):
            for kk in range(2):
                prs.append((dr, kk, dr * 4 + kk))
        return prs
    c0 = t0 // 128
    for sc in range(c0):
        prs.append((0, sc, None))
    for kk in range(4):
        prs.append((0, c0 + kk, kk))
    for kk in range(4):
        prs.append((1, c0 + kk, 4 + kk))
    for sc in range(c0 + 4, NCH):
        prs.append((1, sc, None))
    for sc in range(2):
        prs.append((1, sc, None))
    return prs


def phase_ssd_b(k, l):
    d = k.d
    with Phase(k.nc, k.pool, "sb%d" % l) as ph:
        BT = ph.sb("BT", [128, 2, T], BF16)
        CT = ph.sb("CT", [128, 2, T], BF16)
        xtm = ph.sb("xtm", [128, NCH, 512], BF16)
        R = ph.sb("R", [40, T])
        bcs = ph.sb("bcs", [128, NCH, 40])
        mk = ph.sb("mk", [128, 8, 512])
        sel = ph.sb("sel", [40, 16 * 128])
        ph.ld(sel[:], d["c32"][0:40, K_SEL:NK], "sel", ["sel"])
        xs = [ph.sb("xs%d" % i, [128, 4, 512]) for i in range(2)]
        zs = [ph.sb("zs%d" % i, [128, 4, 512]) for i in range(2)]
        yz = ph.sb("yz", [128, 4, 512])
        sq = Ring([(ph.sb("sq%d" % i, [128, 512]), "sq%d" % i) for i in range(2)])
        rs = ph.sb("rs", [128, 512])
        Lt = Ring([(ph.sb("Lt%d" % i, [128, 512]), "Lt%d" % i) for i in range(4)])
        tm = Ring([(ph.sb("tm%d" % i, [128, 512]), "tm%d" % i) for i in range(2)])
        Mt = Ring([(ph.sb("Mt%d" % i, [128, 512], BF16), "Mt%d" % i) for i in range(4)])
        ost = Ring([(ph.sb("ost%d" % i, [128, 512], BF16), "ost%d" % i) for i in range(3)])
        psY = Ring([(ph.ps("psY%d" % i, [128, 512]), "psY%d" % i) for i in range(2)])
        psRr = [(ph.ps("psR%d" % i, [128, 512]), "psR%d" % i) for i in range(2)]
        psG = Ring([(ph.ps("psG%d" % i, [128, 512]), "psG%d" % i) for i in range(3)])
        psN = (ph.ps("psN", [128, 512]), "psN")
        for g in range(2):
            ph.ld(BT[:, g, :], d["BT"][g], "BT", ["BT"])
            ph.ld(CT[:, g, :], d["CT"][g], "CT", ["CT"])
        xv = d["x_tm"].rearrange("(c p) f -> p c f", p=128)
        ph.ld(xtm[:, 0:17], xv[:, 0:17], "xtm", ["xtm"])
        ph.ld(xtm[:, 17:34], xv[:, 17:34], "xtm", ["xtm"])
        ph.ld(R[:], d["Rrow"], "R", ["R"])
        ph.ld(bcs[:], d["bcol"].rearrange("(c p) r -> p c r", p=128), "bcs", ["bcs"])
        ph.ld(mk[:], d["masks"].rearrange("m p t -> p m t"), "mk", ["mk"])
        xsv = d["xsT"].rearrange("(c p) t -> p c t", p=128)
        zv = d["zT"].rearrange("(c p) t -> p c t", p=128)

        def load_t(ti):
            t0, W = TILES[ti]
            b = ti % 2
            ph.ld(xs[b][:, :, :W], xsv[:, :, t0:t0 + W], "xs%d" % b, ["xs%d" % b])
            ph.ld(zs[b][:, :, :W], zv[:, :, t0:t0 + W], "zs%d" % b, ["zs%d" % b])

        load_t(0)
        ones32 = k.c32[:, K_ONES:K_ONES + 128]
        for ti, (t0, W) in enumerate(TILES):
            b = ti % 2
            if ti + 1 < len(TILES):
                load_t(ti + 1)
            prs = ssd_pairs(ti)
            for cch in range(4):
                g = cch // 2
                py, pyn = psY.next()
                for hh in range(2):
                    h = cch * 2 + hh
                    for dr in range(2):
                        j = dr * 8 + h
                        ph.mm(psRr[dr][0][:, :W], sel[0:40, j * 128:(j + 1) * 128], R[0:40, t0:t0 + W], True, True, ["sel", "R"], [psRr[dr][1]])
                    pend = {}

                    def stA(pi, h=h, g=g, W=W, t0=t0):
                        dr, sc, mi = prs[pi]
                        row = h if dr == 0 else 32 + h
                        pg, pgn = psG.next()
                        ph.mm(pg[:, :W], BT[:, g, sc * 128:(sc + 1) * 128], CT[:, g, t0:t0 + W], True, True, ["BT", "CT"], [pgn])
                        bcol = bcs[:, sc, row:row + 1]
                        L, Ln_ = Lt.next()
                        if mi is None:
                            ph.act(L[:, :W], psRr[dr][0][:, :W], AF.Exp, [psRr[dr][1], "bcs"], [Ln_], bias=bcol)
                        else:
                            t_, tn = tm.next()
                            ph.stt(t_[:, :W], psRr[dr][0][:, :W], bcol, mk[:, mi, :W], ALU.add, ALU.add, [psRr[dr][1], "bcs", "mk"], [tn])
                            ph.act(L[:, :W], t_[:, :W], AF.Exp, [tn], [Ln_])
                        M, Mn = Mt.next()
                        ph.tt("vector", M[:, :W], pg[:, :W], L[:, :W], ALU.mult, [pgn, Ln_], [Mn])
                        pend[pi] = (M, Mn, sc)

                    def stB(pi, h=h, hh=hh, W=W, py=py, pyn=pyn):
                        M, Mn, sc = pend.pop(pi)
                        ph.mm(py[hh * 64:(hh + 1) * 64, :W], xtm[:, sc, h * 64:(h + 1) * 64], M[:, :W], pi == 0, pi == len(prs) - 1, ["xtm", Mn], [pyn])

                    stA(0)
                    if len(prs) > 1:
                        stA(1)
                    for pi in range(len(prs)):
                        if pi + 2 < len(prs):
                            stA(pi + 2)
                        stB(pi)
                s_, sn = sq.next()
                ph.stt(s_[:, :W], xs[b][:, cch, :W], k.cols[:, C_SD + cch:C_SD + cch + 1], py[:, :W], ALU.mult, ALU.add, ["xs%d" % b, "cols", pyn], [sn])
                ph.tt("gpsimd", yz[:, cch, :W], s_[:, :W], zs[b][:, cch, :W], ALU.mult, [sn, "zs%d" % b], ["yz%d" % cch])
                ph.act(s_[:, :W], yz[:, cch, :W], AF.Square, ["yz%d" % cch], [sn])
                ph.mm(psN[0][:, :W], ones32, s_[:, :W], cch == 0, cch == 3, ["c32", sn], [psN[1]])
            ph.act(rs[:, :W], psN[0][:, :W], AF.Sqrt, [psN[1]], ["rs"], bias=k.epsc[:, 0:1], scale=1.0 / 512)
            ph.recip(rs[:, :W], rs[:, :W], ["rs"], ["rs"])
            for cch in range(4):
                o, on = ost.next()
                ph.stt(o[:, :W], yz[:, cch, :W], k.cols[:, C_SN + cch:C_SN + cch + 1], rs[:, :W], ALU.mult, ALU.mult, ["yz%d" % cch, "cols", "rs"], [on])
                ph.store(d["featT"][1536 + cch * 128:1536 + (cch + 1) * 128, t0:t0 + W], o[:, :W], on, [on])


def phase_out(k, l, xsrc, tiles):
    d = k.d
    with Phase(k.nc, k.pool, "out%d" % l) as ph:
        wo = ph.sb("wo", [128, 16, D], BF16)
        fts = [ph.sb("ft0", [128, 16, 512], BF16)] * 2
        xts = [ph.sb("xt%d" % i, [128, 16, 512]) for i in range(2)]
        tmp = ph.sb("tmp", [128, 16, 512])
        rs = ph.sb("rs", [128, 512])
        rw = ph.sb("rw", [128, 16, NE])
        rb = ph.sb("rb", [128, NE])
        lgt = Ring([tuple((ph.sb("lg%d_%d" % (i, j), [128, NE]), "lg%d_%d" % (i, j)) for j in range(3)) for i in range(2)])
        m8 = Ring([(ph.sb("m8_%d" % i, [128, 8]), "m8_%d" % i) for i in range(2)])
        sm = Ring([(ph.sb("sm%d" % i, [128, 2]), "sm%d" % i) for i in range(2)])
        gts = ph.sb("gts", [NE, 512])
        psA = Ring([(ph.ps("psA%d" % i, [128, 512]), "psA%d" % i) for i in range(4)])
        psn = (ph.ps("psn", [128, 512]), "psn")
        psl = Ring([(ph.ps("psl%d" % i, [128, NE]), "psl%d" % i) for i in range(2)])
        pst = (ph.ps("pst", [NE, 512]), "pst")
        wsrc = d["w_out"][l].rearrange("(kc p) f -> p kc f", p=128)
        for q4 in range(4):
            ph.ld(wo[:, 4 * q4:4 * q4 + 4, :], wsrc[:, 4 * q4:4 * q4 + 4, :], "wo", ["wo"], eng="gpsimd")
        ph.ld(rw[:], d["router_w"][l].rearrange("(kc p) e -> p kc e", p=128), "rw", ["rw"])
        ph.ld(rb[:], d["rb"][l], "rb", ["rb"])
        fv = d["featT"].rearrange("(c p) t -> p c t", p=128)
        xv = xsrc.rearrange("(c p) t -> p c t", p=128)
        xo = d["xw"].rearrange("(c p) t -> p c t", p=128)
        hv = d["h2T"].rearrange("(c p) t -> p c t", p=128)

        def load_t(i):
            t0, W = tiles[i]
            b = i % 2
            ph.ld(xts[b][:, :, :W], xv[:, :, t0:t0 + W], "xt%d" % b, ["xt%d" % b])

        load_t(0)
        for i, (t0, W) in enumerate(tiles):
            b = i % 2
            v = 1 if t0 == 0 else 0
            ph.ld(fts[b][:, :, :W], fv[:, :, t0:t0 + W], "ft0", ["ft0"])
            if i + 1 < len(tiles):
                load_t(i + 1)
            ft, ftn, xt, xtn = fts[b], "ft0", xts[b], "xt%d" % b
            for dc in range(16):
                p, pn = psA.next()
                for kc in range(16):
                    ph.mm(p[:, :W], wo[:, kc, dc * 128:(dc + 1) * 128], ft[:, kc, :W], kc == 0, kc == 15, ["wo", ftn], [pn])
                ph.stt(xt[:, dc, :W], p[:, :W], k.modsb[:, 32 + dc, v:v + 1], xt[:, dc, :W], ALU.mult, ALU.add, [pn, "modsb", xtn], [xtn])
            ph.store(xo[:, :, t0:t0 + W], xt[:, :, :W], xtn, [xtn])
            norm_tile(ph, k, xt, xtn, W, v, 64, 48, ft, "ft0", tmp, "tmp", rs, "rs", psn[0], psn[1])
            ph.store(hv[:, :, t0:t0 + W], ft[:, :, :W], "ft0", ["ft0"])
            for tc in range(W // 128):
                pl, pln = psl.next()
                for kc in range(16):
                    ph.mm(pl[:, :], tmp[:, kc, tc * 128:(tc + 1) * 128], rw[:, kc, :], kc == 0, kc == 15, ["tmp", "rw"], [pln])
                (lg, lgn), (ex, exn), (mkk, mkn) = lgt.next()
                m, mn = m8.next()
                s2, s2n = sm.next()
                ph.tt("vector", lg[:], pl[:], rb[:], ALU.add, [pln, "rb"], [lgn])
                ph.op("vector", lambda e, m=m, lg=lg: e.max(out=m[:], in_=lg[:]), [lgn], [mn])
                ph.ts("vector", mkk[:], lg[:], m[:, 3:4], None, ALU.is_ge, None, [lgn, mn], [mkn])
                ph.ts("vector", s2[:, 0:1], m[:, 0:1], -1.0, None, ALU.mult, None, [mn], [s2n])
                ph.act(ex[:], lg[:], AF.Exp, [lgn, s2n], [exn], bias=s2[:, 0:1])
                ph.tt("vector", ex[:], ex[:], mkk[:], ALU.mult, [exn, mkn], [exn])
                ph.op("vector", lambda e, s2=s2, ex=ex: e.reduce_sum(out=s2[:, 1:2], in_=ex[:], axis=AX.X), [exn], [s2n])
                ph.recip(s2[:, 1:2], s2[:, 1:2], [s2n], [s2n])
                ph.ts("vector", ex[:], ex[:], s2[:, 1:2], None, ALU.mult, None, [exn, s2n], [exn])
                ph.tr(pst[0][:, tc * 128:(tc + 1) * 128], ex[:], k.c32[:, K_ID:K_ID + 128], [exn, "c32"], [pst[1]])
            ph.cp("vector", gts[:, :W], pst[0][:, :W], [pst[1]], ["gts"])
            ph.store(d["gatesT"][:, t0:t0 + W], gts[:, :W], "gts", ["gts"])


def phase_cast(k, l):
    d = k.d
    with Phase(k.nc, k.pool, "cast%d" % l) as ph:
        for e in range(NE):
            src = d["exp_w_gu"][l, e].rearrange("k (j two) -> k two j", two=2)
            for h in range(2):
                for q in range(4):
                    ph.dma("gpsimd", lambda en, e=e, h=h, q=q, src=src: en.dma_start(
                        out=d["wgu16"][e, h, q * 512:(q + 1) * 512, :], in_=src[q * 512:(q + 1) * 512, h, :],
                        allow_slow_non_contiguous=True), "cast%d" % ((e * 8 + h * 4 + q) % 8))
            for q in range(2):
                ph.dma("gpsimd", lambda en, e=e, q=q: en.dma_start(
                    out=d["wdn16"][e, q * 384:(q + 1) * 384, :], in_=d["exp_w_dn"][l, e, q * 384:(q + 1) * 384, :]),
                    "castd%d" % ((e * 2 + q) % 4))


def phase_moe(k, l, supers):
    d = k.d
    with Phase(k.nc, k.pool, "moe%d" % l) as ph:
        h2 = ph.sb("h2", [128, 16, 1024], BF16)
        acc = ph.sb("acc", [128, 16, 1024])
        wg = [ph.sb("wg%d" % i, [128, 16, 512], BF16) for i in range(2)]
        wd = [ph.sb("wd%d" % i, [128, 2, D], BF16) for i in range(2)]
        Gb = [ph.sb("Gb%d" % i, [128, 1024]) for i in range(2)]
        bdn = ph.sb("bdn", [NE, D])
        gT = ph.sb("gT", [NE, 1024])
        tmps = Ring([tuple((ph.sb("t%d_%d" % (i, j), [128, 512]), "t%d_%d" % (i, j)) for j in range(4)) for i in range(2)])
        actT = Ring([(ph.sb("a%d" % i, [128, 2, 512], BF16), "a%d" % i) for i in range(3)])
        xc = Ring([(ph.sb("xc%d" % i, [128, 512]), "xc%d" % i) for i in range(3)])
        psg = Ring([(ph.ps("psg%d" % i, [128, 512]), "psg%d" % i) for i in range(2)])
        psu = Ring([(ph.ps("psu%d" % i, [128, 512]), "psu%d" % i) for i in range(2)])
        psd = Ring([(ph.ps("psd%d" % i, [128, 512]), "psd%d" % i) for i in range(4)])
        bu1 = ph.sb("bu1", [128, 192])
        ph.ts("vector", bu1[:], k.cols[:, C_BU:C_BU + 192], 1.0, None, ALU.add, None, ["cols"], ["bu1"])
        ph.ld(bdn[:], d["exp_b_dn"][l], "bdn", ["bdn"])
        hv = d["h2T"].rearrange("(c p) t -> p c t", p=128)
        xw = d["xw"].rearrange("(c p) t -> p c t", p=128)
        units = [(e, jp) for e in range(NE) for jp in range(3)]

        for (s0, Ws) in supers:
            tts = [(o, min(512, Ws - o)) for o in range(0, Ws, 512)]
            ph.ld(h2[:, 0:8, :Ws], hv[:, 0:8, s0:s0 + Ws], "h2", ["h2"])
            ph.ld(h2[:, 8:16, :Ws], hv[:, 8:16, s0:s0 + Ws], "h2", ["h2"])
            ph.ld(gT[:, :Ws], d["gatesT"][:, s0:s0 + Ws], "gT", ["gT"])
            for (o, W) in tts:
                for dc in range(16):
                    p, pn = psd.next()
                    ph.mm(p[:, :W], bdn[:, dc * 128:(dc + 1) * 128], gT[:, o:o + W], True, True, ["bdn", "gT"], [pn])
                    ph.cp("scalar", acc[:, dc, o:o + W], p[:, :W], [pn], ["acc%d" % dc])

            def load_unit(ui):
                e, jp = units[ui]
                b = ui % 2
                src = d["exp_w_gu"][l, e].rearrange("(kc p) f -> p kc f", p=128)
                ph.ld(wg[b][:, 0:8, :], src[:, 0:8, jp * 512:(jp + 1) * 512], "wg%d" % b, ["wg%d" % b], eng="gpsimd")
                ph.ld(wg[b][:, 8:16, :], src[:, 8:16, jp * 512:(jp + 1) * 512], "wg%d" % b, ["wg%d" % b], eng="gpsimd")
                ph.ld(wd[b][:], d["exp_w_dn"][l, e].rearrange("(j p) f -> p j f", p=128)[:, 2 * jp:2 * jp + 2, :], "wd%d" % b, ["wd%d" % b], eng="gpsimd")
                if jp == 0:
                    ph.ld(Gb[e % 2][:, :Ws], d["gatesT"][e, s0:s0 + Ws].partition_broadcast(128), "Gb%d" % (e % 2), ["Gb%d" % (e % 2)])

            items = [(ui, ti) for ui in range(len(units)) for ti in range(len(tts))]
            pend = {}

            def stGU(idx):
                ui, ti = items[idx]
                e, jp = units[ui]
                o, W = tts[ti]
                b = ui % 2
                wgb, wgn = wg[b], "wg%d" % b
                gb, gbn = Gb[e % 2], "Gb%d" % (e % 2)
                at, atn = actT.next()
                for jl in range(2):
                    jc = 2 * jp + jl
                    pg, pgn = psg.next()
                    pu, pun = psu.next()
                    for kc in range(16):
                        ph.mm(pg[:, :W], wgb[:, kc, jl * 256:(jl + 1) * 256:2], h2[:, kc, o:o + W], kc == 0, kc == 15, [wgn, "h2"], [pgn])
                        if kc % 4 == 3:
                            yield
                    for kc in range(16):
                        ph.mm(pu[:, :W], wgb[:, kc, jl * 256 + 1:(jl + 1) * 256:2], h2[:, kc, o:o + W], kc == 0, kc == 15, [wgn, "h2"], [pun])
                        if kc % 4 == 3:
                            yield
                    (g1, g1n), (sg, sgn), (u1, u1n), (t1, t1n) = tmps.next()
                    cb = e * 6 + jc
                    ph.ts("vector", g1[:, :W], pg[:, :W], k.cols[:, C_BG + cb:C_BG + cb + 1], 7.0, ALU.add, ALU.min, [pgn, "cols"], [g1n])
                    ph.act(sg[:, :W], g1[:, :W], AF.Sigmoid, [g1n], [sgn], scale=1.702)
                    ph.ts("vector", u1[:, :W], pu[:, :W], bu1[:, cb:cb + 1], 8.0, ALU.add, ALU.min, [pun, "bu1"], [u1n])
                    ph.stt(t1[:, :W], u1[:, :W], -6.0, g1[:, :W], ALU.max, ALU.mult, [u1n, g1n], [t1n])
                    ph.tt("gpsimd", t1[:, :W], t1[:, :W], sg[:, :W], ALU.mult, [t1n, sgn], [t1n])
                    ph.tt("gpsimd", at[:, jl, :W], t1[:, :W], gb[:, o:o + W], ALU.mult, [t1n, gbn], [atn])
                pend[idx] = (at, atn)

            def stD(idx):
                ui, ti = items[idx]
                o, W = tts[ti]
                b = ui % 2
                wdb, wdn = wd[b], "wd%d" % b
                at, atn = pend.pop(idx)
                for dc in range(16):
                    p, pn = psd.next()
                    for jl in range(2):
                        ph.mm(p[:, :W], wdb[:, jl, dc * 128:(dc + 1) * 128], at[:, jl, :W], jl == 0, jl == 1, [wdn, atn], [pn])
                    ph.tt("vector", acc[:, dc, o:o + W], acc[:, dc, o:o + W], p[:, :W], ALU.add, ["acc%d" % dc, pn], ["acc%d" % dc])
                    yield

            load_unit(0)
            load_unit(1)
            for _ in stGU(0):
                pass
            for idx in range(len(items)):
                if idx + 1 < len(items):
                    for _ in stGU(idx + 1):
                        pass
                for _ in stD(idx):
                    pass
                ui, ti = items[idx]
                if ti == len(tts) - 1 and ui + 2 < len(units):
                    load_unit(ui + 2)
            for (o, W) in tts:
                a0, a1 = s0 + o, s0 + o + W
                segs = [(0, W, 0)] if a0 >= NCTX else ([(0, W, 1)] if a1 <= NCTX else [(0, NCTX - a0, 1), (NCTX - a0, W, 0)])
                for dc in range(16):
                    x_, xn_ = xc.next()
                    ph.ld(x_[:, :W], xw[:, dc, a0:a1], xn_, [xn_])
                    for (c0, c1, v) in segs:
                        ph.stt(x_[:, c0:c1], acc[:, dc, o + c0:o + c1], k.modsb[:, 80 + dc, v:v + 1], x_[:, c0:c1], ALU.mult, ALU.add, ["acc%d" % dc, "modsb", xn_], [xn_])
                    ph.store(xw[:, dc, a0:a1], x_[:, :W], xn_, [xn_])


IN_SPECS = [
    ("xT", [D, T], F32), ("cvec", [128, 16, 2], F32), ("cols", [DEPTH, 128, NCOLS], F32), ("rb", [DEPTH, 128, NE], F32),
    ("w_mod", [DEPTH, D, 6 * D], F32), ("w_in", [DEPTH, D, D_IN], F32), ("w_out", [DEPTH, D, D], F32),
    ("lru_wa", [DEPTH, 2, 4, 128, 128], F32), ("lru_wx", [DEPTH, 2, 4, 128, 128], F32),
    ("router_w", [DEPTH, D, NE], F32), ("exp_w_gu", [DEPTH, NE, D, 2 * EFF], F32), ("exp_w_dn", [DEPTH, NE, EFF, D], F32),
    ("exp_b_dn", [DEPTH, NE, D], F32),
    ("c32", [128, NK], F32), ("masks", [8, 128, 512], F32), ("rope", [2, 128, T], F32),
    ("cs128", [128, 256], BF16), ("dftL", [2, SEQ, SEQ], BF16), ("dftc", [2, NCTX, NCTX], BF16),
]
SCRATCH = [
    ("hT", [D, T], BF16), ("qT", [512, T], BF16), ("kT", [256, T], BF16), ("v", [T, 256], BF16),
    ("lxT", [512, T], F32), ("lgT", [512, T], F32), ("fT", [512, T], BF16), ("zT", [512, T], F32),
    ("xbcT", [1024, T], F32), ("dtT", [16, T], F32), ("featT", [D, T], BF16), ("PQ", [T, 4, 256], BF16),
    ("xsT", [512, T], F32), ("x_tm", [T, 512], BF16), ("BT", [2, 128, T], BF16), ("CT", [2, 128, T], BF16),
    ("Rrow", [40, T], F32), ("bcol", [T, 40], F32), ("h2T", [D, T], BF16), ("gatesT", [NE, T], F32),
]


def build_program(layers=range(DEPTH), phases=None, dbg=()):
    nc = bass.Bass("TRN2", target_bir_lowering=False)
    k = K()
    k.nc = nc
    k.d = {}
    for name, shape, dt in IN_SPECS:
        k.d[name] = nc.dram_tensor(name, shape, dt, kind="ExternalInput").ap()
    for name, shape, dt in SCRATCH:
        kind = "ExternalOutput" if name in dbg else "Internal"
        k.d[name] = nc.dram_tensor(name, shape, dt, kind=kind).ap()
    k.d["xw"] = nc.dram_tensor("xw", [D, T], F32, kind="ExternalOutput").ap()
    with ExitStack() as st:
        k.pool = SemPool(nc, st)
        k.cols = st.enter_context(nc.sbuf_tensor("sb_cols", [128, NCOLS], F32))
        k.modsb = st.enter_context(nc.sbuf_tensor("sb_mod", [128, 96, 2], F32))
        k.c32 = st.enter_context(nc.sbuf_tensor("sb_c32", [128, K_SEL], F32))
        k.epsc = st.enter_context(nc.sbuf_tensor("sb_epsc", [128, 1], F32))
        k.onec = st.enter_context(nc.sbuf_tensor("sb_onec", [128, 1], F32))
        with Phase(nc, k.pool, "init") as ph:
            ph.ld(k.c32[:], k.d["c32"][:, 0:K_SEL], "c32", ["c32"])
            ph.memset("vector", k.epsc[:], EPS, ["epsc"])
            ph.memset("vector", k.onec[:], 1.0, ["onec"])
        for l in layers:
            last = l == DEPTH - 1
            xsrc = k.d["xT"] if l == 0 else k.d["xw"]
            tiles = TILES[1:] if last else TILES
            supers = ([] if last else [(0, 256)]) + [(256 + 1024 * i, 1024) for i in range(4)]

            def on(p):
                return phases is None or p in phases
            if on("mod"):
                phase_mod(k, l)
            if on("norm1"):
                phase_norm1(k, l, xsrc)
            if on("proj"):
                phase_proj(k, l)
            if on("att"):
                phase_att(k, l)
            if on("lru"):
                phase_lru(k, l)
            if on("four"):
                phase_four1(k, l)
                phase_four2(k, l)
            if on("ssd"):
                phase_ssd_a(k, l)
                phase_ssd_b(k, l)
            if on("out"):
                phase_out(k, l, xsrc, tiles)
            if on("moe"):
                phase_moe(k, l, supers)
    return nc


def _col(vec):
    vec = np.asarray(vec, np.float32)
    return np.ascontiguousarray(vec.reshape(-1, 128).T)


def host_constants():
    c = {}
    c32 = np.zeros((128, NK), np.float32)
    c32[:, K_ONES:K_ONES + 128] = 1.0
    rm = np.zeros((128, 128), np.float32)
    for dd in range(64):
        rm[dd + 64, dd] = -1.0
        rm[dd, dd + 64] = 1.0
    c32[:, K_RM:K_RM + 128] = rm
    c32[:, K_ID:K_ID + 128] = np.eye(128, dtype=np.float32)
    for j in range(16):
        row = j if j < 8 else 32 + (j - 8)
        c32[row, K_SEL + j * 128:K_SEL + (j + 1) * 128] = 1.0
    c["c32"] = c32
    s = np.arange(128)[:, None]
    t = np.arange(512)[None, :]
    masks = np.zeros((8, 128, 512), np.float32)
    for kk in range(4):
        masks[kk] = np.where(kk * 128 + s <= t, 0.0, -1e30)
        masks[4 + kk] = np.where(kk * 128 + s >= t, 0.0, -1e30)
    c["masks"] = masks
    rows = SEQ // 64
    row = np.repeat(np.arange(rows), 64).astype(np.float32)
    colp = np.tile(np.arange(64), rows).astype(np.float32)
    inv = (10000.0 ** (-np.arange(32, dtype=np.float32) / 32)).astype(np.float32)
    ang = np.concatenate([row[:, None] * inv, colp[:, None] * inv], axis=-1)
    ang = np.concatenate([ang, ang], axis=-1).astype(np.float32)
    rope = np.zeros((2, 128, T), np.float32)
    rope[0, :, :NCTX] = 1.0
    rope[0, :, NCTX:] = np.cos(ang).T
    rope[1, :, NCTX:] = np.sin(ang).T
    c["rope"] = rope
    bf = ml_dtypes.bfloat16
    a = 2 * np.pi * np.outer(np.arange(128), np.arange(128)) / 128.0
    c["cs128"] = (np.concatenate([np.cos(a), np.sin(a)], axis=1) * 128 ** -0.5).astype(np.float32).astype(bf)

    def dft(n):
        idx = (np.outer(np.arange(n, dtype=np.int64), np.arange(n, dtype=np.int64)) % n).astype(np.float64)
        ang_ = 2 * np.pi * idx / n
        out = np.empty((2, n, n), bf)
        out[0] = (np.cos(ang_) * n ** -0.5).astype(np.float32).astype(bf)
        out[1] = (-np.sin(ang_) * n ** -0.5).astype(np.float32).astype(bf)
        return out
    c["dftL"] = dft(SEQ)
    c["dftc"] = dft(NCTX)
    return c


def host_shared(inp):
    sh = {}
    cols = np.zeros((DEPTH, 128, NCOLS), np.float32)
    f = lambda name: np.asarray(inp[name], np.float32)
    for l in range(DEPTH):
        cols[l, :, C_BMOD:C_BMOD + 96] = _col(f("b_mod")[l])
        cols[l, :, C_N1:C_N1 + 16] = _col(f("norm1")[l])
        cols[l, :, C_N2:C_N2 + 16] = _col(f("norm2")[l])
        cols[l, :, C_QN] = f("q_norm")[l]
        cols[l, :, C_KN] = f("k_norm")[l]
        lcw = f("lru_conv_w")[l]
        for n in range(4):
            for j in range(4):
                cols[l, :, C_LCW + 4 * n + j] = lcw[j, n * 128:(n + 1) * 128]
        cols[l, :, C_LCB:C_LCB + 4] = _col(f("lru_conv_b")[l])
        for dr in range(2):
            cols[l, :, C_LBA + dr * 4:C_LBA + dr * 4 + 4] = _col(f("lru_ba")[l, dr])
            cols[l, :, C_LBX + dr * 4:C_LBX + dr * 4 + 4] = _col(f("lru_bx")[l, dr])
            cols[l, :, C_LLAM + dr * 4:C_LLAM + dr * 4 + 4] = _col(f("lru_lam")[l, dr])
        scw = f("ssd_conv_w")[l]
        for n in range(8):
            for j in range(4):
                cols[l, :, C_SCW + 4 * n + j] = scw[j, n * 128:(n + 1) * 128]
        cols[l, :, C_SCB:C_SCB + 8] = _col(f("ssd_conv_b")[l])
        cols[l, :, C_SD:C_SD + 4] = _col(np.repeat(f("ssd_d")[l], 64))
        cols[l, :, C_SN:C_SN + 4] = _col(f("ssd_norm")[l])
        for dr in range(2):
            cols[l, dr * 32:dr * 32 + 8, C_ALOG] = f("ssd_a_log")[l, dr]
            cols[l, dr * 32:dr * 32 + 8, C_DTB] = f("ssd_dt_bias")[l, dr]
        bgu = f("exp_b_gu")[l]
        for e in range(NE):
            cols[l, :, C_BG + e * 6:C_BG + e * 6 + 6] = _col(bgu[e, 0::2])
            cols[l, :, C_BU + e * 6:C_BU + e * 6 + 6] = _col(bgu[e, 1::2])
    sh["cols"] = cols
    sh["rb"] = np.ascontiguousarray(np.broadcast_to(f("router_b")[:, None, :], (DEPTH, 128, NE)))
    for name in ("w_mod", "w_in", "w_out", "lru_wa", "lru_wx", "router_w", "exp_w_gu", "exp_w_dn", "exp_b_dn"):
        sh[name] = np.ascontiguousarray(f(name))
    return sh


def host_core(inp, b):
    x = np.asarray(inp["x"][b], np.float32)
    ctx = np.asarray(inp["ctx"][b], np.float32)
    xT = np.ascontiguousarray(np.concatenate([ctx, x], axis=0).T)
    cvec = np.stack([_col(np.asarray(inp["c"][b], np.float32)), _col(np.asarray(inp["c_ctx"], np.float32))], axis=-1)
    return {"xT": xT, "cvec": np.ascontiguousarray(cvec)}


def kernel(**inputs):
    B = inputs["x"].shape[0]
    nc = build_program()
    shared = host_shared(inputs)
    shared.update(host_constants())
    in_maps = []
    for b in range(B):
        m = dict(shared)
        m.update(host_core(inputs, b))
        in_maps.append(m)
    res = run_bass_kernel_spmd(nc, in_maps, core_ids=list(range(B)))
    out = np.empty((B, SEQ, D), np.float32)
    for b in range(B):
        out[b] = np.asarray(res.results[b]["xw"])[:, NCTX:].T
    return out
```
